# Optimizing a Trainium2 kernel written in Bass

```python
import jax, jax.numpy as jnp
from jax import lax
import numpy as np

D_MODEL = 1024
BATCH = 2
SEQ = 8192
DEPTH = 1

PLE_DIM = 256
HEAD_DIM = 64
N_ATTN_HEADS = 8
D_ATTN = N_ATTN_HEADS * HEAD_DIM
D_CONV = D_MODEL // 2
D_MIX = D_ATTN + D_CONV
D_IN = 3 * D_ATTN + 2 * D_CONV
CONV_WIDTH = 31
ROPE_DIM = HEAD_DIM // 4
ROPE_THETA = 500000.0
DILATED_PATTERNS = ((128, 1), (512, 4), (2048, 16))
ATTN_BLOCK = 128
N_KEYS = 128
N_EXPERTS = N_KEYS * N_KEYS
PEER_HEADS = 8
PEER_TOPK = 16
D_KEY = 256
PEER_BLOCK = 128
EPS = 1e-6

kernel_name = "hymba_dilated_conformer_peer_layer"


def _rmsnorm(x, g):
    xf = x.astype(jnp.float32)
    y = xf * lax.rsqrt(jnp.mean(xf * xf, axis=-1, keepdims=True) + EPS)
    return (y * g.astype(jnp.float32)).astype(x.dtype)


def _layernorm(x, g, b):
    xf = x.astype(jnp.float32)
    mu = jnp.mean(xf, axis=-1, keepdims=True)
    var = jnp.mean(jnp.square(xf - mu), axis=-1, keepdims=True)
    y = (xf - mu) * lax.rsqrt(var + EPS)
    return (y * g.astype(jnp.float32) + b.astype(jnp.float32)).astype(x.dtype)


def _partial_rope(t, positions):
    inv_freq = ROPE_THETA ** (-jnp.arange(0, ROPE_DIM, 2, dtype=jnp.float32) / ROPE_DIM)
    ang = positions.astype(jnp.float32)[..., None] * inv_freq
    cos = jnp.cos(ang)[:, :, None, :]
    sin = jnp.sin(ang)[:, :, None, :]
    tr = t[..., :ROPE_DIM].astype(jnp.float32)
    t1, t2 = tr[..., :ROPE_DIM // 2], tr[..., ROPE_DIM // 2:]
    rot = jnp.concatenate([t1 * cos - t2 * sin, t2 * cos + t1 * sin], axis=-1).astype(t.dtype)
    return jnp.concatenate([rot, t[..., ROPE_DIM:]], axis=-1)


def _dilated_attention(q, k, v, window, dilation):
    B, S, H, Dh = q.shape
    span = window // dilation
    L = S // dilation
    nb = -(-L // ATTN_BLOCK)
    Lp = nb * ATTN_BLOCK

    def to_sub(t):
        t = t.reshape(B, L, dilation, H, Dh).transpose(0, 2, 3, 1, 4)
        return jnp.pad(t, ((0, 0), (0, 0), (0, 0), (0, Lp - L), (0, 0)))

    def windows(t):
        t = jnp.pad(t, ((0, 0), (0, 0), (0, 0), (ATTN_BLOCK, 0), (0, 0)))
        t = t.reshape(B, dilation, H, nb + 1, ATTN_BLOCK, Dh)
        return jnp.concatenate([t[:, :, :, :-1], t[:, :, :, 1:]], axis=4)

    qb = to_sub(q).reshape(B, dilation, H, nb, ATTN_BLOCK, Dh)
    kw = windows(to_sub(k))
    vw = windows(to_sub(v))
    qi = jnp.arange(ATTN_BLOCK)[:, None]
    kj = jnp.arange(2 * ATTN_BLOCK)[None, :]
    blk = jnp.arange(nb)[:, None, None]
    dist = ATTN_BLOCK + qi - kj
    key_pos = blk * ATTN_BLOCK + kj - ATTN_BLOCK
    mask = (dist >= 0) & (dist <= span) & (key_pos >= 0)
    s = jnp.einsum('bdhnqe,bdhnke->bdhnqk', qb, kw,
                   preferred_element_type=jnp.float32) * (Dh ** -0.5)
    s = jnp.where(mask, s, -jnp.inf)
    m = jnp.max(s, axis=-1, keepdims=True)
    e = jnp.exp(s - m)
    l = jnp.sum(e, axis=-1, keepdims=True)
    o = jnp.einsum('bdhnqk,bdhnke->bdhnqe', e.astype(vw.dtype), vw,
                   preferred_element_type=jnp.float32) / l
    lse = (m + jnp.log(l))[..., 0]

    def from_sub(t):
        t = t.reshape(B, dilation, H, Lp, *t.shape[5:])[:, :, :, :L]
        t = jnp.moveaxis(t, 3, 1)
        return t.reshape(B, S, H, *t.shape[4:])

    return from_sub(o), from_sub(lse)


def _token_mixers(xn, positions, w_in, conv_w, conv_b, conv_ln_g, conv_ln_b,
                  g_attn_out, g_conv_out, w_out):
    B, S, _ = xn.shape
    z = xn @ w_in
    q, k, v, a, gt = jnp.split(z, [D_ATTN, 2 * D_ATTN, 3 * D_ATTN, 3 * D_ATTN + D_CONV], axis=-1)
    heads = lambda t: t.reshape(B, S, N_ATTN_HEADS, HEAD_DIM)
    q = _partial_rope(heads(q), positions)
    k = _partial_rope(heads(k), positions)
    v = heads(v)
    outs, lses = zip(*[_dilated_attention(q, k, v, w, d) for (w, d) in DILATED_PATTERNS])
    alpha = jax.nn.softmax(jnp.stack(lses, axis=0), axis=0)
    attn = jnp.sum(alpha[..., None] * jnp.stack(outs, axis=0), axis=0)
    attn = attn.reshape(B, S, D_ATTN).astype(xn.dtype)
    glu = a * jax.nn.sigmoid(gt)
    c = lax.conv_general_dilated(glu, conv_w[:, None, :], window_strides=(1,),
                                 padding=[(CONV_WIDTH - 1, 0)],
                                 dimension_numbers=('NWC', 'WIO', 'NWC'),
                                 feature_group_count=D_CONV) + conv_b
    c = jax.nn.silu(_layernorm(c, conv_ln_g, conv_ln_b))
    y = jnp.concatenate([_rmsnorm(attn, g_attn_out), _rmsnorm(c, g_conv_out)], axis=-1)
    return y @ w_out


def _peer(xn, w_q, sub_keys, expert_u, expert_v):
    B, S, D = xn.shape
    N = B * S
    HK = PEER_HEADS * PEER_TOPK
    xt = xn.reshape(N, D)
    q = (xt @ w_q).reshape(N, PEER_HEADS, 2, D_KEY // 2)
    s = jnp.einsum('nhpc,pkc->nhpk', q, sub_keys, preferred_element_type=jnp.float32)
    s_half, i_half = lax.top_k(s, PEER_TOPK)
    cand = (s_half[:, :, 0, :, None] + s_half[:, :, 1, None, :]).reshape(
        N, PEER_HEADS, PEER_TOPK * PEER_TOPK)
    top_s, top_c = lax.top_k(cand, PEER_TOPK)
    i1 = jnp.take_along_axis(i_half[:, :, 0], top_c // PEER_TOPK, axis=-1)
    i2 = jnp.take_along_axis(i_half[:, :, 1], top_c % PEER_TOPK, axis=-1)
    nblk = N // PEER_BLOCK
    expert = (i1 * N_KEYS + i2).reshape(nblk, PEER_BLOCK, HK)
    gate = jax.nn.softmax(top_s, axis=-1).reshape(nblk, PEER_BLOCK, HK)

    def block(args):
        xb, eb, gb = args
        h = jnp.einsum('td,tkd->tk', xb, expert_u[eb], preferred_element_type=jnp.float32)
        act = (jax.nn.gelu(h, approximate=False) * gb).astype(xb.dtype)
        return jnp.einsum('tk,tkd->td', act, expert_v[eb])

    y = lax.map(block, (xt.reshape(nblk, PEER_BLOCK, D), expert, gate))
    return y.reshape(B, S, D)


def setup_inputs(seed: int = 0) -> dict:
    key = jax.random.key(seed)
    ks = jax.random.split(key, 20)
    nrm = lambda k, shape, scale: jax.random.normal(k, shape, jnp.float32) * scale
    gain = lambda k, shape: 1.0 + 0.02 * jax.random.normal(k, shape, jnp.float32)
    return {
        "x": nrm(ks[0], (BATCH, SEQ, D_MODEL), 1.0),
        "p": nrm(ks[1], (DEPTH, BATCH, SEQ, PLE_DIM), 1.0),
        "positions": jnp.broadcast_to(jnp.arange(SEQ, dtype=jnp.int32), (BATCH, SEQ)),
        "norm_mix": gain(ks[2], (DEPTH, D_MODEL)),
        "w_in": nrm(ks[3], (DEPTH, D_MODEL, D_IN), D_MODEL ** -0.5),
        "conv_w": nrm(ks[4], (DEPTH, CONV_WIDTH, D_CONV), CONV_WIDTH ** -0.5),
        "conv_b": nrm(ks[5], (DEPTH, D_CONV), 0.02),
        "conv_ln_g": gain(ks[6], (DEPTH, D_CONV)),
        "conv_ln_b": nrm(ks[7], (DEPTH, D_CONV), 0.02),
        "g_attn_out": gain(ks[8], (DEPTH, D_ATTN)),
        "g_conv_out": gain(ks[9], (DEPTH, D_CONV)),
        "w_out": nrm(ks[10], (DEPTH, D_MIX, D_MODEL), D_MIX ** -0.5),
        "norm_ffn": gain(ks[11], (DEPTH, D_MODEL)),
        "peer_wq": nrm(ks[12], (DEPTH, D_MODEL, PEER_HEADS * D_KEY), D_MODEL ** -0.5),
        "sub_keys": nrm(ks[13], (DEPTH, 2, N_KEYS, D_KEY // 2), (D_KEY // 2) ** -0.5),
        "expert_u": nrm(ks[14], (DEPTH, N_EXPERTS, D_MODEL), D_MODEL ** -0.5),
        "expert_v": nrm(ks[15], (DEPTH, N_EXPERTS, D_MODEL), PEER_HEADS ** -0.5),
        "norm_ple": gain(ks[16], (DEPTH, D_MODEL)),
        "w_ple_gate": nrm(ks[17], (DEPTH, D_MODEL, D_MODEL), D_MODEL ** -0.5),
        "w_ple_proj": nrm(ks[18], (DEPTH, PLE_DIM, D_MODEL), PLE_DIM ** -0.5),
        "final_norm": gain(ks[19], (D_MODEL,)),
    }


def reference(x, p, positions, norm_mix, w_in, conv_w, conv_b, conv_ln_g, conv_ln_b,
              g_attn_out, g_conv_out, w_out, norm_ffn, peer_wq, sub_keys, expert_u,
              expert_v, norm_ple, w_ple_gate, w_ple_proj, final_norm):
    h = x
    for i in range(DEPTH):
        h = h + _token_mixers(_rmsnorm(h, norm_mix[i]), positions, w_in[i], conv_w[i], conv_b[i],
                              conv_ln_g[i], conv_ln_b[i], g_attn_out[i], g_conv_out[i], w_out[i])
        h = h + _peer(_rmsnorm(h, norm_ffn[i]), peer_wq[i], sub_keys[i], expert_u[i], expert_v[i])
        ple_gate = jax.nn.sigmoid(_rmsnorm(h, norm_ple[i]) @ w_ple_gate[i])
        h = h + ple_gate * (p[i] @ w_ple_proj[i])
    return _rmsnorm(h, final_norm)
```

```python
import contextlib
import numpy as np
import concourse.bass as bass
import concourse.mybir as mybir
from concourse.bass_utils import run_bass_kernel_spmd

F32 = mybir.dt.float32
BF16 = mybir.dt.bfloat16
I32 = mybir.dt.int32
U32 = mybir.dt.uint32
AF = mybir.ActivationFunctionType
ALU = mybir.AluOpType
AX = mybir.AxisListType

NCORES = 8
D = 1024
TOK = 2048
HALO = 2048
NT = TOK // 128
NTH = (TOK + HALO) // 128
EPS = 1e-6
PATTERNS = ((128, 1), (512, 4), (2048, 16))
TWO_PI = 6.283185307179586
C1 = 6.28125
C2 = TWO_PI - C1


class Prog:
    ENGS = ['pe', 'act', 'dve', 'pool', 'sp']

    def __init__(self, nc, tag):
        self.nc = nc
        self.tag = tag
        self.ops = {e: [] for e in self.ENGS}
        self.cnt = {}
        self.lastw = {}
        self.readers = {}
        self.seen = {e: {} for e in self.ENGS}
        self.excl = set()

    def _deps(self, e, reads, writes):
        deps = {}

        def add(d):
            sn, v = d
            if deps.get(sn, 0) < v:
                deps[sn] = v
        for r in reads:
            if r in self.lastw:
                add(self.lastw[r])
        for w in writes:
            if w in self.lastw:
                add(self.lastw[w])
            for rd in self.readers.get(w, ()):
                add(rd)
        out = []
        for sn, v in deps.items():
            if self.seen[e].get(sn, 0) < v:
                self.seen[e][sn] = v
                out.append((sn, v))
        return out

    LIMIT = None
    NOPS = 0
    SEMSTACK = None

    def op(self, e, fn, reads=(), writes=(), sem=None, inc=1, pe_chain=False):
        Prog.NOPS += 1
        if Prog.LIMIT is not None and Prog.NOPS > Prog.LIMIT:
            return None
        semname = sem or ('S_' + e)
        writes = list(writes) + [r for r in reads if r in self.excl and r not in writes]
        waits = self._deps(e, reads, writes)
        if pe_chain:
            waits = [(sn, v) for (sn, v) in waits if sn != 'S_pe']
        self.cnt[semname] = self.cnt.get(semname, 0) + inc
        val = self.cnt[semname]

        def emit(eng, sems, waits=waits, fn=fn, semname=semname, inc=inc):
            for (sn, v) in waits:
                eng.wait_ge(sems[sn], v)
            fn(eng).then_inc(sems[semname], inc)
        self.ops[e].append(emit)
        for w in writes:
            self.lastw[w] = (semname, val)
            self.readers[w] = []
        for r in reads:
            if r not in writes:
                self.readers.setdefault(r, []).append((semname, val))
        return (semname, val)

    def dma(self, q, chan, out, in_, reads=(), writes=(), chain=False, **kw):
        if chain:
            writes = list(writes) + ['chain_' + chan]
        return self.op(q, lambda eng: eng.dma_start(out=out, in_=in_, **kw), reads, writes,
                       sem='D_' + chan, inc=16)

    def emit(self):
        nc = self.nc
        names = sorted(self.cnt.keys())
        with contextlib.ExitStack() as st:
            sems = {n: Prog.SEMSTACK.enter_context(nc.semaphore(self.tag + n)) for n in names}
            block = st.enter_context(nc.Block())
            hooks = {'pe': block.tensor, 'act': block.scalar, 'dve': block.vector,
                     'pool': block.gpsimd, 'sp': block.sync}
            for e in self.ENGS:
                ops = self.ops[e]
                extra = [(n, self.cnt[n]) for n in names]

                def body(eng, ops=ops, extra=extra):
                    for o in ops:
                        o(eng, sems)
                    for (n, v) in extra:
                        eng.wait_ge(sems[n], v)
                hooks[e](body)


class Deferred:
    def __init__(self, P):
        self.P = P
        self.q = []

    def op(self, *a, **k):
        self.q.append(('op', a, k))

    def dma(self, *a, **k):
        self.q.append(('dma', a, k))

    def flush(self, n=None):
        m = len(self.q) if n is None else min(n, len(self.q))
        for kind, a, k in self.q[:m]:
            getattr(self.P, kind)(*a, **k)
        del self.q[:m]


def bc(ap, shape):
    return ap.to_broadcast(list(shape))


def build_program(debug=None):
    nc = bass.Bass("TRN2", target_bir_lowering=False)
    dt_in = lambda name, shape, dt=F32: nc.dram_tensor(name, list(shape), dt, kind="ExternalInput").ap()
    xh = dt_in("xh", [NTH * 128, D])
    pos_tm = dt_in("pos_tm", [128, NTH], I32)
    hv_d = dt_in("hv", [128, 1])
    invf_d = dt_in("invf", [128, 8])
    ident_d = dt_in("ident", [128, 128])
    w_in = dt_in("w_in", [D, 2560])
    g_mix = dt_in("g_mix", [128, 8])
    mask2_d = dt_in("mask2", [128, 2, 128])
    cw_d = dt_in("cw", [128, 4, 31])
    convb_d = dt_in("convb_r", [128, 512])
    lng_d = dt_in("lng_r", [128, 512])
    lnb_d = dt_in("lnb_r", [128, 512])
    g_y = dt_in("g_y", [128, 8])
    w_out = dt_in("w_out", [D, D])
    out_d = nc.dram_tensor("out", [TOK, D], F32, kind="ExternalOutput").ap()
    od = nc.dram_tensor("od", [3, TOK, 520], F32, kind="Internal").ap()
    h1d = nc.dram_tensor("h1d", [TOK, D], F32, kind="Internal").ap()
    if debug == 'B':
        dbg_h1 = nc.dram_tensor("dbg_h1", [TOK, D], F32, kind="ExternalOutput").ap()
    if debug == 'D1':
        dbg_e = nc.dram_tensor("dbg_eidx", [128, 128], I32, kind="ExternalOutput").ap()
        dbg_a = nc.dram_tensor("dbg_actg", [128, 128], F32, kind="ExternalOutput").ap()
        dbg_h = nc.dram_tensor("dbg_hcol", [128, 128], F32, kind="ExternalOutput").ap()
        dbg_g = nc.dram_tensor("dbg_gate", [128, 128], F32, kind="ExternalOutput").ap()
        dbg_y = nc.dram_tensor("dbg_h2", [128, 1024], F32, kind="ExternalOutput").ap()
    p_own = dt_in("p_own", [TOK, 256])
    w_pq = dt_in("w_pq", [D, 2048])
    skT_d = dt_in("skT", [128, 2, 128])
    w_g = dt_in("w_g", [D, D])
    w_p = dt_in("w_p", [256, D])
    g_ffn = dt_in("g_ffn_r", [128, D])
    g_ple = dt_in("g_ple", [128, 8])
    fn_d = dt_in("fn_r", [128, D])
    iota_d = dt_in("iota16", [128, 16])
    euv = dt_in("euv", [16384, 2 * D])
    euvb = nc.dram_tensor("euvb", [16384, 2 * D], BF16, kind="Internal").ap()
    vd = nc.dram_tensor("vd", [NTH * 128, 520], BF16, kind="Internal").ap()
    dbg = {}
    if debug == 'A':
        dbg['qT'] = nc.dram_tensor("dbg_qT", [128, 2, 4, TOK], BF16, kind="ExternalOutput").ap()
        dbg['kT'] = nc.dram_tensor("dbg_kT", [128, 4, NTH * 128], BF16, kind="ExternalOutput").ap()
        dbg['gluT'] = nc.dram_tensor("dbg_gluT", [128, 4, 128 + TOK], BF16, kind="ExternalOutput").ap()
        dbg['vd'] = nc.dram_tensor("dbg_vd", [NTH * 128, 520], BF16, kind="ExternalOutput").ap()

    with contextlib.ExitStack() as outer:
        Prog.SEMSTACK = outer
        sb = lambda name, shape, dt=F32: outer.enter_context(nc.sbuf_tensor(name, list(shape), dt))
        ident_f = sb("ident_f", [128, 128])
        ident_b = sb("ident_b", [128, 128], BF16)
        hv = sb("hv_sb", [128, 1])
        mid = contextlib.ExitStack()
        mid.__enter__()
        sbm = lambda name, shape, dt=F32: mid.enter_context(nc.sbuf_tensor(name, list(shape), dt))
        qT = sbm("qT", [128, 2, 4, TOK], BF16)
        kT = sbm("kT", [128, 4, NTH * 128], BF16)
        gluT = sbm("gluT", [128, 4, 128 + TOK], BF16)

        with contextlib.ExitStack() as pa:
            sa = lambda name, shape, dt=F32: pa.enter_context(nc.sbuf_tensor(name, list(shape), dt))
            P = Prog(nc, "A")

            def ps(name, shape, dt=F32):
                P.excl.add(name)
                return pa.enter_context(nc.psum_tensor(name, list(shape), dt))
            W = sa("W_in_bf", [128, 8, 2560], BF16)
            gmix = sa("gmix", [128, 8])
            pos_i = sa("pos_i", [128, NTH], I32)
            pos_f = sa("pos_f", [128, NTH])
            invf = sa("invf_sb", [128, 8])
            ang = sa("ang", [128, NTH, 8])
            sinT = sa("sinT", [128, NTH, 8])
            cosT = sa("cosT", [128, NTH, 8])
            tmpa = sa("tmpa", [128, NTH, 8])
            tmpn = sa("tmpn", [128, NTH, 8])
            tmpi = sa("tmpi", [128, NTH, 8], I32)
            xt = [sa(f"xt{i}", [128, D]) for i in range(2)]
            junk = sa("junk", [128, D], BF16)
            ss = [sa(f"ss{i}", [128, 1]) for i in range(2)]
            rt = [sa(f"rt{i}", [128, 1]) for i in range(2)]
            rstd = [sa(f"rstd{i}", [128, 1]) for i in range(2)]
            xs = [sa(f"xs{i}", [128, D], BF16) for i in range(2)]
            xnT = [sa(f"xnT{i}", [128, 8, 128], BF16) for i in range(2)]
            zc = sa("zc", [128, 1024])
            zb = sa("zb", [128, 1024], BF16)
            rtmp = [sa(f"rtmp{i}", [128, 16, 8]) for i in range(4)]
            vt = [sa(f"vt{i}", [128, 8, 65], BF16) for i in range(2)]
            sg = sa("sg", [128, 512])
            glu = sa("glu", [128, 512], BF16)
            pst = [ps(f"pst{i}", [128, 8, 128], BF16) for i in range(2)]
            ps_qk = ps("ps_qk", [128, 1024])
            ps_v = ps("ps_v", [128, 512])
            ps_ag = ps("ps_ag", [128, 1024])
            pst2 = ps("pst2", [128, 8, 128], BF16)

            P.op('pool', lambda e: e.memset(qT[:, 0, :, :], 0.0), writes=['qT'])
            P.op('pool', lambda e: e.memset(qT[:, 1, :, :], 0.0), reads=['qT'], writes=['qT'])
            P.dma('sp', 'constA', ident_f[:], ident_d[:, :], writes=['ident_f'], chain=True)
            P.op('dve', lambda e: e.tensor_copy(out=ident_b[:], in_=ident_f[:]), reads=['ident_f'], writes=['ident_b'])
            P.dma('sp', 'constA', hv[:], hv_d[:, :], writes=['hv'], chain=True)
            P.dma('sp', 'constA', gmix[:], g_mix[:, :], writes=['gmix'], chain=True)
            P.dma('sp', 'constA', pos_i[:], pos_tm[:, :], writes=['pos_i'], chain=True)
            P.dma('sp', 'constA', invf[:], invf_d[:, :], writes=['invf'], chain=True)
            for c in range(8):
                for hh in range(2):
                    P.dma('pool', 'Wld', W[:, c, hh * 1280:(hh + 1) * 1280],
                          w_in[c * 128:(c + 1) * 128, hh * 1280:(hh + 1) * 1280], writes=[f'W{c}'], chain=True)
            P.op('dve', lambda e: e.tensor_copy(out=pos_f[:], in_=pos_i[:]), reads=['pos_i'], writes=['pos_f'])
            P.op('dve', lambda e: e.tensor_tensor(out=ang[:], in0=bc(pos_f[:].unsqueeze(2), [128, NTH, 8]),
                                                  in1=bc(invf[:].unsqueeze(1), [128, NTH, 8]), op=ALU.mult),
                 reads=['pos_f', 'invf'], writes=['ang'])

            def sin_of(dst, shift, tagn):
                P.op('dve', lambda e: e.tensor_scalar(out=tmpa[:], in0=ang[:], scalar1=float(shift), scalar2=None,
                                                      op0=ALU.add), reads=['ang'], writes=['tmpa'])
                P.op('dve', lambda e: e.tensor_scalar(out=tmpn[:], in0=tmpa[:], scalar1=1.0 / TWO_PI, scalar2=0.5,
                                                      op0=ALU.mult, op1=ALU.add), reads=['tmpa'], writes=['tmpn'])
                P.op('dve', lambda e: e.tensor_copy(out=tmpi[:], in_=tmpn[:]), reads=['tmpn'], writes=['tmpi'])
                P.op('dve', lambda e: e.tensor_copy(out=tmpn[:], in_=tmpi[:]), reads=['tmpi'], writes=['tmpn'])
                P.op('dve', lambda e: e.scalar_tensor_tensor(out=tmpa[:], in0=tmpn[:], scalar=-C1, in1=tmpa[:],
                                                             op0=ALU.mult, op1=ALU.add),
                     reads=['tmpn', 'tmpa'], writes=['tmpa'])
                P.op('dve', lambda e: e.scalar_tensor_tensor(out=tmpa[:], in0=tmpn[:], scalar=-C2, in1=tmpa[:],
                                                             op0=ALU.mult, op1=ALU.add),
                     reads=['tmpn', 'tmpa'], writes=['tmpa'])
                P.op('dve', lambda e: e.tensor_scalar(out=tmpn[:], in0=tmpa[:], scalar1=-np.pi, scalar2=TWO_PI,
                                                      op0=ALU.is_lt, op1=ALU.mult), reads=['tmpa'], writes=['tmpn'])
                P.op('dve', lambda e: e.tensor_tensor(out=tmpa[:], in0=tmpa[:], in1=tmpn[:], op=ALU.add),
                     reads=['tmpa', 'tmpn'], writes=['tmpa'])
                P.op('dve', lambda e: e.tensor_scalar(out=tmpn[:], in0=tmpa[:], scalar1=np.pi, scalar2=-TWO_PI,
                                                      op0=ALU.is_gt, op1=ALU.mult), reads=['tmpa'], writes=['tmpn'])
                P.op('dve', lambda e: e.tensor_tensor(out=tmpa[:], in0=tmpa[:], in1=tmpn[:], op=ALU.add),
                     reads=['tmpa', 'tmpn'], writes=['tmpa'])
                P.op('dve', lambda e: e.tensor_scalar(out=tmpa[:], in0=tmpa[:], scalar1=-3.14159, scalar2=3.14159,
                                                      op0=ALU.max, op1=ALU.min), reads=['tmpa'], writes=['tmpa'])
                P.op('act', lambda e: e.activation(out=dst[:], in_=tmpa[:], func=AF.Sin), reads=['tmpa'], writes=[tagn])
            sin_of(sinT, 0.0, 'sinT')
            sin_of(cosT, np.pi / 2, 'cosT')

            class Router:
                target = None

                def op(self, *a, **k):
                    return self.target.op(*a, **k)

                def dma(self, *a, **k):
                    return self.target.dma(*a, **k)
            R = Router()
            S1 = [Deferred(P) for _ in range(NTH)]
            S2 = [Deferred(P) for _ in range(NTH)]
            def rope(nh, col0, i):
                zv = zc[:, col0:col0 + nh * 64].rearrange("p (h e) -> p h e", e=64)
                bv = zb[:, col0:col0 + nh * 64].rearrange("p (h e) -> p h e", e=64)
                cs = bc(cosT[:, i, :].unsqueeze(1), [128, nh, 8])
                sn = bc(sinT[:, i, :].unsqueeze(1), [128, nh, 8])
                z1, z2 = zv[:, :, 0:8], zv[:, :, 8:16]
                t = [r[:, 0:nh, :] for r in rtmp]
                R.op('pool', lambda e: e.tensor_tensor(out=t[0], in0=z1, in1=cs, op=ALU.mult),
                     reads=['zc', 'cosT'], writes=['rt0'])
                R.op('pool', lambda e: e.tensor_tensor(out=t[1], in0=z2, in1=sn, op=ALU.mult),
                     reads=['zc', 'sinT'], writes=['rt1'])
                R.op('pool', lambda e: e.tensor_tensor(out=t[2], in0=z2, in1=cs, op=ALU.mult),
                     reads=['zc', 'cosT'], writes=['rt2'])
                R.op('pool', lambda e: e.tensor_tensor(out=t[3], in0=z1, in1=sn, op=ALU.mult),
                     reads=['zc', 'sinT'], writes=['rt3'])
                R.op('dve', lambda e: e.tensor_copy(out=zb[:, col0:col0 + nh * 64], in_=zc[:, col0:col0 + nh * 64]),
                     reads=['zc'], writes=['zb'])
                R.op('dve', lambda e: e.tensor_tensor(out=bv[:, :, 0:8], in0=t[0], in1=t[1], op=ALU.subtract),
                     reads=['rt0', 'rt1', 'zb'], writes=['zb'])
                R.op('dve', lambda e: e.tensor_tensor(out=bv[:, :, 8:16], in0=t[2], in1=t[3], op=ALU.add),
                     reads=['rt2', 'rt3', 'zb'], writes=['zb'])

            for i in range(NTH):
                b = i % 2
                R.target = S1[i]
                own = i >= NT
                R.dma('sp', f'xt{b}', xt[b][:], xh[i * 128:(i + 1) * 128, :], writes=[f'xt{b}'])
                R.op('act', lambda e, b=b: e.activation(out=junk[:], in_=xt[b][:], func=AF.Square, accum_out=ss[b][:]),
                     reads=[f'xt{b}'], writes=['junk', f'ss{b}'])
                R.op('act', lambda e, b=b: e.activation(out=rt[b][:], in_=ss[b][:], func=AF.Sqrt, scale=1.0 / D, bias=EPS),
                     reads=[f'ss{b}'], writes=[f'rt{b}'])
                R.op('dve', lambda e, b=b: e.reciprocal(out=rstd[b][:], in_=rt[b][:]), reads=[f'rt{b}'], writes=[f'rstd{b}'])
                R.op('dve', lambda e, b=b: e.tensor_scalar(out=xs[b][:], in0=xt[b][:], scalar1=rstd[b][:, 0:1], scalar2=None,
                                                           op0=ALU.mult), reads=[f'xt{b}', f'rstd{b}'], writes=[f'xs{b}'])
                for c in range(8):
                    R.op('pe', lambda e, b=b, c=c: e.transpose(out=pst[b][:, c, :], in_=xs[b][:, c * 128:(c + 1) * 128],
                                                               identity=ident_b[:]),
                         reads=[f'xs{b}', 'ident_b'], writes=[f'pst{b}'], pe_chain=True)
                R.op('dve', lambda e, b=b: e.tensor_tensor(out=xnT[b][:], in0=pst[b][:],
                                                           in1=bc(gmix[:].unsqueeze(2), [128, 8, 128]), op=ALU.mult),
                     reads=[f'pst{b}', 'gmix'], writes=[f'xnT{b}'])
                R.target = S2[i]

                def slab(psum_ap, col0, tag, b=b):
                    for c in range(8):
                        R.op('pe', lambda e, c=c: e.matmul(psum_ap, lhsT=xnT[b][:, c, :], rhs=W[:, c, col0:col0 + 512],
                                                          start=(c == 0), stop=(c == 7)),
                             reads=[f'xnT{b}', f'W{c}'], writes=[tag], pe_chain=True)
                if own:
                    slab(ps_qk[:, 0:512], 0, 'ps_qk')
                slab(ps_qk[:, 512:1024], 512, 'ps_qk')
                slab(ps_v[:, :], 1024, 'ps_v')
                conv_tile = i >= NT - 1
                if conv_tile:
                    slab(ps_ag[:, 0:512], 1536, 'ps_ag')
                    slab(ps_ag[:, 512:1024], 2048, 'ps_ag')
                lo = 0 if own else 512
                nh = 16 if own else 8
                R.op('act', lambda e, lo=lo: e.copy(out=zc[:, lo:1024], in_=ps_qk[:, lo:1024]), reads=['ps_qk'], writes=['zc'])
                vb = i % 2
                pv3 = ps_v[:, :].rearrange("p (h e) -> p h e", e=64)
                if own:
                    R.op('act', lambda e, vb=vb: e.activation(out=vt[vb][:, :, 0:64], in_=pv3, func=AF.Copy), reads=['ps_v'], writes=[f'vt{vb}'])
                    R.op('pool', lambda e, vb=vb: e.memset(vt[vb][:, :, 64:65], 1.0), reads=[f'vt{vb}'], writes=[f'vt{vb}'])
                else:
                    R.op('act', lambda e, vb=vb: e.activation(out=vt[vb][:, :, 0:64], in_=pv3, func=AF.Copy, scale=hv[:, 0:1]),
                         reads=['ps_v', 'hv'], writes=[f'vt{vb}'])
                    R.op('dve', lambda e, vb=vb: e.tensor_copy(out=vt[vb][:, :, 64:65], in_=bc(hv[:].unsqueeze(1), [128, 8, 1])),
                         reads=[f'vt{vb}', 'hv'], writes=[f'vt{vb}'])
                R.dma('sp', f'vd{vb}', vd[i * 128:(i + 1) * 128, :], vt[vb][:].rearrange("p h e -> p (h e)"),
                      reads=[f'vt{vb}'], writes=['vd'])
                if conv_tile:
                    R.op('act', lambda e: e.activation(out=sg[:], in_=ps_ag[:, 512:1024], func=AF.Sigmoid),
                         reads=['ps_ag'], writes=['sg'])
                    R.op('dve', lambda e: e.tensor_tensor(out=glu[:], in0=ps_ag[:, 0:512], in1=sg[:], op=ALU.mult),
                         reads=['ps_ag', 'sg'], writes=['glu'])
                rope(nh, lo, i)
                c0 = 0 if own else 4
                for c in range(c0, 8):
                    R.op('pe', lambda e, c=c: e.transpose(out=pst2[:, c, :], in_=zb[:, c * 128:(c + 1) * 128], identity=ident_b[:]),
                         reads=['zb', 'ident_b'], writes=['pst2'], pe_chain=True)
                if own:
                    R.op('act', lambda e, i=i: e.activation(out=qT[0:64, 0, :, (i - NT) * 128:(i - NT + 1) * 128], in_=pst2[0:64, 0:4, :], func=AF.Copy),
                         reads=['pst2'], writes=['qT'])
                    R.op('act', lambda e, i=i: e.activation(out=qT[64:128, 1, :, (i - NT) * 128:(i - NT + 1) * 128], in_=pst2[64:128, 0:4, :], func=AF.Copy),
                         reads=['pst2', 'qT'], writes=['qT'])
                R.op('dve', lambda e, i=i: e.tensor_copy(out=kT[:, :, i * 128:(i + 1) * 128], in_=pst2[:, 4:8, :]),
                     reads=['pst2'], writes=['kT'])
                if conv_tile:
                    for c in range(4):
                        R.op('pe', lambda e, c=c: e.transpose(out=pst2[:, c, :], in_=glu[:, c * 128:(c + 1) * 128], identity=ident_b[:]),
                             reads=['glu', 'ident_b'], writes=['pst2'], pe_chain=True)
                    j = i - (NT - 1)
                    R.op('act', lambda e, j=j: e.copy(out=gluT[:, :, j * 128:(j + 1) * 128], in_=pst2[:, 0:4, :]),
                         reads=['pst2'], writes=['gluT'])
            S1[0].flush()
            for i in range(NTH):
                if i + 1 < NTH:
                    S1[i + 1].flush()
                S2[i].flush()
            if debug == 'A':
                P.dma('sp', 'dq', dbg['qT'][:, :, :, :], qT[:], reads=['qT'], writes=['dq'])
                P.dma('sp', 'dk', dbg['kT'][:, :, :], kT[:], reads=['kT'], writes=['dk'])
                P.dma('sp', 'dg', dbg['gluT'][:, :, :], gluT[:], reads=['gluT'], writes=['dg'])
                P.dma('sp', 'dv', dbg['vd'][:, :], vd[:, :], reads=['vd'], writes=['dv'])
            P.emit()
        if debug == 'A':
            mid.__exit__(None, None, None)
            return nc

        with contextlib.ExitStack() as pb:
            sa = lambda name, shape, dt=F32: pb.enter_context(nc.sbuf_tensor(name, list(shape), dt))
            P = Prog(nc, "B")

            def ps(name, shape, dt=F32):
                P.excl.add(name)
                return pb.enter_context(nc.psum_tensor(name, list(shape), dt))
            vp = sa("vp", [128, 32, 520], BF16)
            mask_f = sa("mask_f", [128, 2, 128])
            mask_b = sa("mask_b", [128, 2, 128], BF16)
            e_sb = [sa(f"e_sb{i}", [128, 4, 2, 128], BF16) for i in range(2)]
            pT = [sa(f"pT{i}", [128, 4, 2, 128], BF16) for i in range(2)]
            o_sb = [sa(f"o_sb{i}", [128, 520]) for i in range(2)]
            ps_s = [ps(f"ps_s{i}", [128, 4, 2, 128]) for i in range(2)]
            ps_o = [ps(f"ps_o{i}", [128, 4, 65]) for i in range(2)]
            P.dma('sp', 'mask', mask_f[:], mask2_d[:, :, :], writes=['mask_f'])
            for r in range(16):
                P.dma('pool', 'ecast', euvb[r * 1024:(r + 1) * 1024, :], euv[r * 1024:(r + 1) * 1024, :], writes=['euvb'], chain=True)
            P.op('dve', lambda e: e.tensor_copy(out=mask_b[:], in_=mask_f[:]), reads=['mask_f'], writes=['mask_b'])
            class RouterB:
                target = None

                def op(self, *a, **k):
                    return self.target.op(*a, **k)

                def dma(self, *a, **k):
                    return self.target.dma(*a, **k)
            RB = RouterB()
            stS, stEM, stPV = [], [], []
            blk = 0
            for pi, (wdw, d) in enumerate(PATTERNS):
                NBK = 32 // d
                vsrc = vd.rearrange("(nbk jk d) f -> jk d nbk f", jk=128, d=d)
                vpv = vp[:].rearrange("p (r nbk) f -> p r nbk f", r=d)
                odv = od[pi].rearrange("(nb i d) f -> i d nb f", i=128, d=d)
                vload = Deferred(P)
                RB.target = P if pi == 0 else vload
                if d == 1:
                    for g in range(4):
                        RB.dma('sp', 'vp', vpv[:, 0, g * 8:(g + 1) * 8, :], vsrc[:, 0, g * 8:(g + 1) * 8, :],
                              reads=['vd'], writes=['vp'])
                else:
                    for r in range(d):
                        RB.dma('sp', 'vp', vpv[:, r, :, :], vsrc[:, r, :, :], reads=['vd'], writes=['vp'])
                for r in range(d):
                    for nbq in range(NBK // 2, NBK):
                        ob = blk % 2
                        blk += 1
                        stS.append(Deferred(P))
                        stEM.append(Deferred(P))
                        stPV.append(Deferred(P))
                        if vload is not None and pi > 0:
                            stPV[-1].q.extend(vload.q)
                            vload.q = []
                        vload = None
                        RB.target = stS[-1]
                        qs = (nbq - NBK // 2) * 128 * d + r
                        for half in range(2):
                            for hh in range(4):
                                h = half * 4 + hh
                                c, po = h // 2, 64 * (h % 2)
                                for kb in range(2):
                                    ks = (nbq - 1 + kb) * 128 * d + r
                                    RB.op('pe', lambda e, half=half, hh=hh, kb=kb, c=c, po=po, ks=ks, qs=qs, d=d: e.matmul(
                                        ps_s[half][:, hh, kb, :], lhsT=kT[:, c, ks:ks + 127 * d + 1:d],
                                        rhs=qT[:, po // 64, c, qs:qs + 127 * d + 1:d], start=True, stop=True),
                                        reads=['kT', 'qT'], writes=[f'ps_s{half}'], pe_chain=True)
                        RB.target = stEM[-1]
                        for half in range(2):
                            RB.op('act', lambda e, half=half: e.activation(out=e_sb[half][:], in_=ps_s[half][:], func=AF.Exp, scale=0.125),
                                 reads=[f'ps_s{half}'], writes=[f'e_sb{half}'])
                            RB.op('dve', lambda e, half=half: e.tensor_tensor(out=pT[half][:], in0=e_sb[half][:],
                                                                             in1=bc(mask_b[:].unsqueeze(1), [128, 4, 2, 128]), op=ALU.mult),
                                 reads=[f'e_sb{half}', 'mask_b'], writes=[f'pT{half}'])
                        RB.target = stPV[-1]
                        for half in range(2):
                            for hh in range(4):
                                h = half * 4 + hh
                                for kb in range(2):
                                    RB.op('pe', lambda e, half=half, hh=hh, kb=kb, h=h, r=r, nbq=nbq, vpv=vpv: e.matmul(
                                        ps_o[half][:, hh, :], lhsT=pT[half][:, hh, kb, :],
                                        rhs=vpv[:, r, nbq - 1 + kb, h * 65:(h + 1) * 65], start=(kb == 0), stop=(kb == 1)),
                                        reads=[f'pT{half}', 'vp'], writes=[f'ps_o{half}'], pe_chain=True)
                        for half in range(2):
                            RB.op('act', lambda e, half=half, ob=ob: e.activation(
                                out=o_sb[ob][:, half * 260:(half + 1) * 260], in_=ps_o[half][:].rearrange("p h e -> p (h e)"), func=AF.Copy),
                                reads=[f'ps_o{half}'], writes=[f'o_sb{ob}'])
                        RB.dma('sp', f'od{ob}', odv[:, r, nbq - NBK // 2, :], o_sb[ob][:], reads=[f'o_sb{ob}'], writes=['od'])
            nblk = len(stS)
            stS[0].flush()
            for bi in range(nblk):
                stEM[bi].flush()
                if bi + 1 < nblk:
                    stS[bi + 1].flush()
                stPV[bi].flush()
            P.emit()

        with contextlib.ExitStack() as pb:
            sa = lambda name, shape, dt=F32: pb.enter_context(nc.sbuf_tensor(name, list(shape), dt))
            P = Prog(nc, "C")

            def ps(name, shape, dt=F32):
                P.excl.add(name)
                return pb.enter_context(nc.psum_tensor(name, list(shape), dt))
            Wo = sa("Wo", [128, 8, D], BF16)
            gy = sa("gy", [128, 8])
            cw = sa("cw_sb", [128, 4, 31])
            diagW = sa("diagW", [128, 4, 31, 128], BF16)
            convb = sa("convb", [128, 512])
            lng = sa("lng", [128, 512])
            lnb = sa("lnb", [128, 512])
            o3 = [sa(f"o3_{i}", [128, 3, 520]) for i in range(2)]
            acc = sa("acc", [128, 8, 65])
            rden = sa("rden", [128, 8])
            attn = sa("attn", [128, 8, 64])
            junk2 = sa("junk2", [128, 512], BF16)
            sm = [sa(f"sm{i}", [128, 1]) for i in range(8)]
            yb = sa("yb", [128, D], BF16)
            cs = sa("cs", [128, 512])
            cs2 = sa("cs2", [128, 512])
            bst = sa("bst", [128, 6])
            mv = sa("mv", [128, 2])
            yT = sa("yT", [128, 8, 128], BF16)
            xr = [sa(f"xr{i}", [128, D]) for i in range(2)]
            h1 = [sa(f"h1_{i}", [128, D]) for i in range(2)]
            ps_c = [ps(f"ps_c{i}", [128, 512]) for i in range(2)]
            ps_t = ps("ps_t", [128, 8, 128], BF16)
            ps_h = ps("ps_h", [128, D])
            P.dma('sp', 'constC', gy[:], g_y[:, :], writes=['gy'], chain=True)
            P.dma('sp', 'constC', cw[:], cw_d[:, :, :], writes=['cw'], chain=True)
            P.dma('sp', 'constC', convb[:], convb_d[:, :], writes=['convb'], chain=True)
            P.dma('sp', 'constC', lng[:], lng_d[:, :], writes=['lng'], chain=True)
            P.dma('sp', 'constC', lnb[:], lnb_d[:, :], writes=['lnb'], chain=True)
            for c in range(8):
                P.dma('pool', 'Wold', Wo[:, c, :], w_out[c * 128:(c + 1) * 128, :], writes=[f'Wo{c}'], chain=True)
            for cc in range(4):
                for j in range(31):
                    eng = 'dve' if (j % 2 == 0) else 'pool'
                    P.op(eng, lambda e, cc=cc, j=j: e.tensor_scalar(out=diagW[:, cc, j, :], in0=ident_f[:], scalar1=cw[:, cc, j:j + 1],
                                                                    scalar2=None, op0=ALU.mult),
                         reads=['ident_f', 'cw'], writes=[f'diagW{cc}_{j}'])

            def rstd_of(P, src_tag, src_ap, n, k0):
                P.op('act', lambda e: e.activation(out=junk2[:, 0:n], in_=src_ap, func=AF.Square, accum_out=sm[k0][:]),
                     reads=[src_tag], writes=['junk2', f'sm{k0}'])
                P.op('act', lambda e: e.activation(out=sm[k0 + 1][:], in_=sm[k0][:], func=AF.Sqrt, scale=1.0 / n, bias=EPS),
                     reads=[f'sm{k0}'], writes=[f'sm{k0 + 1}'])
                P.op('dve', lambda e: e.reciprocal(out=sm[k0 + 2][:], in_=sm[k0 + 1][:]), reads=[f'sm{k0 + 1}'], writes=[f'sm{k0 + 2}'])

            def stageC(X, t, which):
                b = t % 2
                if which == 1:
                    X.dma('sp', f'o3_{b}', o3[b][:], od[:, t * 128:(t + 1) * 128, :].rearrange("a p f -> p a f"),
                          reads=['od'], writes=[f'o3_{b}'])
                    X.dma('sp', f'xr{b}', xr[b][:], xh[HALO + t * 128:HALO + (t + 1) * 128, :], writes=[f'xr{b}'])
                    for cc in range(4):
                        for j in range(31):
                            col = 128 + t * 128 - 30 + j
                            X.op('pe', lambda e, cc=cc, j=j, col=col, b=b: e.matmul(ps_c[b][:, cc * 128:(cc + 1) * 128], lhsT=gluT[:, cc, col:col + 128],
                                                                              rhs=diagW[:, cc, j, :], start=(j == 0), stop=(j == 30)),
                                 reads=['gluT', f'diagW{cc}_{j}'], writes=[f'ps_c{b}'], pe_chain=True)

                    return
                accf = acc[:].rearrange("p h e -> p (h e)")
                X.op('dve', lambda e, b=b: e.tensor_tensor(out=accf, in0=o3[b][:, 0, :], in1=o3[b][:, 1, :], op=ALU.add),
                     reads=[f'o3_{b}'], writes=['acc'])
                X.op('dve', lambda e, b=b: e.tensor_tensor(out=accf, in0=accf, in1=o3[b][:, 2, :], op=ALU.add),
                     reads=[f'o3_{b}', 'acc'], writes=['acc'])
                X.op('dve', lambda e: e.reciprocal(out=rden[:], in_=acc[:, :, 64]), reads=['acc'], writes=['rden'])
                X.op('dve', lambda e: e.tensor_tensor(out=attn[:], in0=acc[:, :, 0:64], in1=bc(rden[:].unsqueeze(2), [128, 8, 64]), op=ALU.mult),
                     reads=['acc', 'rden'], writes=['attn'])
                attf = attn[:].rearrange("p h e -> p (h e)")
                rstd_of(X, 'attn', attf, 512, 0)
                X.op('dve', lambda e: e.tensor_scalar(out=yb[:, 0:512], in0=attf, scalar1=sm[2][:, 0:1], scalar2=None, op0=ALU.mult),
                     reads=['attn', 'sm2'], writes=['yb'])
                X.op('dve', lambda e, b=b: e.tensor_tensor(out=cs[:], in0=ps_c[b][:], in1=convb[:], op=ALU.add),
                     reads=[f'ps_c{b}', 'convb'], writes=['cs'])
                X.op('dve', lambda e: e.bn_stats(out=bst[:], in_=cs[:]), reads=['cs'], writes=['bst'])
                X.op('dve', lambda e: e.bn_aggr(out=mv[:], in_=bst[:]), reads=['bst'], writes=['mv'])
                X.op('act', lambda e: e.activation(out=sm[6][:], in_=mv[:, 1:2], func=AF.Sqrt, scale=1.0, bias=EPS),
                     reads=['mv'], writes=['sm6'])
                X.op('dve', lambda e: e.reciprocal(out=sm[7][:], in_=sm[6][:]), reads=['sm6'], writes=['sm7'])
                X.op('dve', lambda e: e.tensor_scalar(out=cs2[:], in0=cs[:], scalar1=mv[:, 0:1], scalar2=sm[7][:, 0:1],
                                                      op0=ALU.subtract, op1=ALU.mult), reads=['cs', 'mv', 'sm7'], writes=['cs2'])
                X.op('pool', lambda e: e.tensor_tensor(out=cs2[:], in0=cs2[:], in1=lng[:], op=ALU.mult), reads=['cs2', 'lng'], writes=['cs2'])
                X.op('pool', lambda e: e.tensor_tensor(out=cs2[:], in0=cs2[:], in1=lnb[:], op=ALU.add), reads=['cs2', 'lnb'], writes=['cs2'])
                X.op('act', lambda e: e.activation(out=cs[:], in_=cs2[:], func=AF.Silu), reads=['cs2'], writes=['cs'])
                rstd_of(X, 'cs', cs[:], 512, 3)
                X.op('dve', lambda e: e.tensor_scalar(out=yb[:, 512:1024], in0=cs[:], scalar1=sm[5][:, 0:1], scalar2=None, op0=ALU.mult),
                     reads=['cs', 'sm5'], writes=['yb'])
                for c in range(8):
                    X.op('pe', lambda e, c=c: e.transpose(out=ps_t[:, c, :], in_=yb[:, c * 128:(c + 1) * 128], identity=ident_b[:]),
                         reads=['yb', 'ident_b'], writes=['ps_t'], pe_chain=True)
                X.op('dve', lambda e: e.tensor_tensor(out=yT[:], in0=ps_t[:], in1=bc(gy[:].unsqueeze(2), [128, 8, 128]), op=ALU.mult),
                     reads=['ps_t', 'gy'], writes=['yT'])
                for hf in range(2):
                    for c in range(8):
                        X.op('pe', lambda e, c=c, hf=hf: e.matmul(ps_h[:, hf * 512:(hf + 1) * 512], lhsT=yT[:, c, :],
                                                                 rhs=Wo[:, c, hf * 512:(hf + 1) * 512], start=(c == 0), stop=(c == 7)),
                             reads=['yT', f'Wo{c}'], writes=['ps_h'], pe_chain=True)
                X.op('dve', lambda e, b=b: e.tensor_tensor(out=h1[b][:], in0=ps_h[:], in1=xr[b][:], op=ALU.add),
                     reads=['ps_h', f'xr{b}'], writes=[f'h1_{b}'])
                X.dma('sp', f'h1d{b}', h1d[t * 128:(t + 1) * 128, :], h1[b][:], reads=[f'h1_{b}'], writes=['h1d'])
            CS1 = [Deferred(P) for _ in range(NT)]
            CS2 = [Deferred(P) for _ in range(NT)]
            for t in range(NT):
                stageC(CS1[t], t, 1)
                stageC(CS2[t], t, 2)
            CS1[0].flush()
            for t in range(NT):
                if t + 1 < NT:
                    CS1[t + 1].flush()
                CS2[t].flush()
            if debug == 'B':
                P.dma('sp', 'dh1', dbg_h1[:, :], h1d[:, :], reads=['h1d'], writes=['dh1'])
            P.emit()
        if debug == 'B':
            mid.__exit__(None, None, None)
            return nc

        mid.__exit__(None, None, None)
        with contextlib.ExitStack() as pd:
            sa = lambda name, shape, dt=F32: pd.enter_context(nc.sbuf_tensor(name, list(shape), dt))
            P = Prog(nc, "D")

            def ps(name, shape, dt=F32):
                P.excl.add(name)
                return pd.enter_context(nc.psum_tensor(name, list(shape), dt))
            NB, NDG = 12, 8
            Wq = sa("Wq", [128, 8, 2048], BF16)
            Wg = sa("Wg", [128, 8, D], BF16)
            Wp = sa("Wp", [128, 2, D], BF16)
            skf = sa("skf", [128, 2, 128])
            skT = sa("skT_sb", [128, 2, 128], BF16)
            gffn = sa("gffn", [128, D])
            fnr = sa("fnr", [128, D])
            gple = sa("gple", [128, 8])
            iota = sa("iota", [128, 16])
            h1t = [sa(f"h1t{i}", [128, D]) for i in range(2)]
            xn2 = sa("xn2", [128, D])
            outt = sa("outt", [128, D])
            junkf = sa("junkf", [128, D], BF16)
            junkb = sa("junkb", [128, D], BF16)
            xn2b = [sa(f"xn2b{i}", [128, D], BF16) for i in range(2)]
            xn2T = sa("xn2T", [128, 8, 128], BF16)
            pqT = sa("pqT", [128, 16, 128], BF16)
            s_sb = sa("s_sb", [128, 2048])
            s2 = sa("s2", [128, 2048])
            cand = sa("cand", [128, 8, 16, 16])
            v16 = sa("v16", [128, 16, 16])
            i16 = sa("i16", [128, 16, 16], U32)
            i16f = sa("i16f", [128, 16, 16])
            ts = sa("ts", [128, 8, 16])
            tc = sa("tc", [128, 8, 16], U32)
            ta = sa("ta", [128, 8, 16], U32)
            tb = sa("tb", [128, 8, 16], U32)
            taf = sa("taf", [128, 8, 16])
            tbf = sa("tbf", [128, 8, 16])
            i1 = sa("i1", [128, 8, 16])
            i2 = sa("i2", [128, 8, 16])
            eidf = sa("eidf", [128, 128])
            eidx = [sa(f"eidx{i}", [128, 128], I32) for i in range(2)]
            eg = sa("eg", [128, 8, 16])
            zz = sa("zz", [128, 8])
            gate = [sa(f"gate{i}", [128, 8, 16]) for i in range(2)]
            hcol = sa("hcol", [128, 8, 16])
            actg = sa("actg", [128, 8, 16])
            sm = [sa(f"dsm{i}", [128, 1]) for i in range(9)]
            junk = sa("junkd", [128, D], BF16)
            uvb = [sa(f"uvb{i}", [128, 2 * D], BF16) for i in range(NB)]
            dg = [sa(f"dg{i}", [128, 128], BF16) for i in range(NDG)]
            h2 = sa("h2", [128, D])
            xn3b = sa("xn3b", [128, D], BF16)
            xn3T = sa("xn3T", [128, 8, 128], BF16)
            sgm = sa("sgm", [128, D])
            pt = [sa(f"pt{i}", [128, 256]) for i in range(3)]
            ptb = sa("ptb", [128, 256], BF16)
            pTT = sa("pTT", [128, 2, 128], BF16)
            ps_t = ps("psd_t", [128, 8, 128], BF16)
            ps_tb = ps("psd_tb", [128, 8, 128], BF16)
            ps_pq = ps("ps_pq", [128, 4, 128])
            ps_sc = ps("ps_sc", [128, 4, 128])
            ps_y = ps("ps_y", [128, D])
            ps_g = ps("ps_g", [128, D])
            P.dma('sp', 'constD', skf[:], skT_d[:, :, :], writes=['skf'], chain=True)
            P.op('dve', lambda e: e.tensor_copy(out=skT[:], in_=skf[:]), reads=['skf'], writes=['skT'])
            P.dma('sp', 'constD', gffn[:], g_ffn[:, :], writes=['gffn'], chain=True)
            P.dma('sp', 'constD', fnr[:], fn_d[:, :], writes=['fnr'], chain=True)
            P.dma('sp', 'constD', gple[:], g_ple[:, :], writes=['gple'], chain=True)
            P.dma('sp', 'constD', iota[:], iota_d[:, :], writes=['iota'], chain=True)
            for c in range(8):
                P.dma('pool', 'WDld', Wq[:, c, :], w_pq[c * 128:(c + 1) * 128, :], writes=[f'Wq{c}'], chain=True)
            for c in range(8):
                P.dma('pool', 'WDld', Wg[:, c, :], w_g[c * 128:(c + 1) * 128, :], writes=[f'Wg{c}'], chain=True)
            for c in range(2):
                P.dma('pool', 'WDld', Wp[:, c, :], w_p[c * 128:(c + 1) * 128, :], writes=[f'Wp{c}'], chain=True)

            def rstd_d(P, src_tag, src_ap, k0, jk, jktag):
                P.op('act', lambda e: e.activation(out=jk[:], in_=src_ap, func=AF.Square, accum_out=sm[k0][:]),
                     reads=[src_tag], writes=[jktag, f'dsm{k0}'])
                P.op('act', lambda e: e.activation(out=sm[k0 + 1][:], in_=sm[k0][:], func=AF.Sqrt, scale=1.0 / D, bias=EPS),
                     reads=[f'dsm{k0}'], writes=[f'dsm{k0 + 1}'])
                P.op('dve', lambda e: e.reciprocal(out=sm[k0 + 2][:], in_=sm[k0 + 1][:]), reads=[f'dsm{k0 + 1}'], writes=[f'dsm{k0 + 2}'])

            def top16(P, src, srctag, scr, scrtag, vout, iout, outtag):
                P.op('dve', lambda e: e.max(out=vout[:, 0:8], in_=src), reads=[srctag], writes=[outtag])
                P.op('dve', lambda e: e.max_index(out=iout[:, 0:8], in_max=vout[:, 0:8], in_values=src),
                     reads=[srctag, outtag], writes=[outtag + 'i'])
                P.op('dve', lambda e: e.match_replace(out=scr, in_to_replace=vout[:, 0:8], in_values=src, imm_value=-1e30),
                     reads=[srctag, outtag], writes=[scrtag])
                P.op('dve', lambda e: e.max(out=vout[:, 8:16], in_=scr), reads=[scrtag, outtag], writes=[outtag])
                P.op('dve', lambda e: e.max_index(out=iout[:, 8:16], in_max=vout[:, 8:16], in_values=scr),
                     reads=[scrtag, outtag, outtag + 'i'], writes=[outtag + 'i'])

            ntiles = {'D1': 1, 'D2': 2, 'D3': 3}.get(debug, NT)
            def front(X, t):
                s = t % 2
                s3 = t % 3
                X.dma('sp', f'h1t{s}', h1t[s][:], h1d[t * 128:(t + 1) * 128, :], reads=['h1d'], writes=[f'h1t{s}'])
                X.dma('sp', f'pt{s3}', pt[s3][:], p_own[t * 128:(t + 1) * 128, :], writes=[f'pt{s3}'])
                rstd_d(X, f'h1t{s}', h1t[s][:], 0, junkf, 'junkf')
                X.op('dve', lambda e, s=s: e.scalar_tensor_tensor(out=xn2[:], in0=h1t[s][:], scalar=sm[2][:, 0:1], in1=gffn[:],
                                                             op0=ALU.mult, op1=ALU.mult), reads=[f'h1t{s}', 'dsm2', 'gffn'], writes=['xn2'])
                X.op('act', lambda e, s=s: e.activation(out=xn2b[s][:], in_=xn2[:], func=AF.Copy), reads=['xn2'], writes=[f'xn2b{s}'])
                for c in range(8):
                    X.op('pe', lambda e, s=s, c=c: e.transpose(out=ps_t[:, c, :], in_=xn2b[s][:, c * 128:(c + 1) * 128], identity=ident_b[:]),
                         reads=[f'xn2b{s}', 'ident_b'], writes=['psd_t'], pe_chain=True)
                X.op('act', lambda e, s=s: e.activation(out=xn2T[:], in_=ps_t[:], func=AF.Copy), reads=['psd_t'], writes=['xn2T'])
                for g in range(4):
                    for q in range(4):
                        hp = g * 4 + q
                        for c in range(8):
                            X.op('pe', lambda e, s=s, c=c, q=q, hp=hp: e.matmul(ps_pq[:, q, :], lhsT=Wq[:, c, hp * 128:(hp + 1) * 128],
                                                                          rhs=xn2T[:, c, :], start=(c == 0), stop=(c == 7)),
                                 reads=['xn2T', f'Wq{c}'], writes=['ps_pq'], pe_chain=True)
                    X.op('act', lambda e, s=s, g=g: e.activation(out=pqT[:, g * 4:(g + 1) * 4, :], in_=ps_pq[:], func=AF.Copy),
                         reads=['ps_pq'], writes=['pqT'])
                    for q in range(4):
                        hp = g * 4 + q
                        X.op('pe', lambda e, s=s, q=q, hp=hp: e.matmul(ps_sc[:, q, :], lhsT=pqT[:, hp, :], rhs=skT[:, hp % 2, :],
                                                                 start=True, stop=True),
                             reads=['pqT', 'skT'], writes=['ps_sc'], pe_chain=True)
                    X.op('act', lambda e, s=s, g=g: e.activation(out=s_sb[:, g * 512:(g + 1) * 512], in_=ps_sc[:].rearrange("p a b -> p (a b)"),
                                                            func=AF.Copy), reads=['ps_sc'], writes=['s_sb'])
                for hp in range(16):
                    top16(X, s_sb[:, hp * 128:(hp + 1) * 128], 's_sb', s2[:, hp * 128:(hp + 1) * 128], 's2',
                          v16[:, hp, :], i16[:, hp, :], 'v16')
                X.op('dve', lambda e, s=s: e.tensor_copy(out=i16f[:], in_=i16[:]), reads=['v16i'], writes=['i16f'])
                v16v = v16[:].rearrange("p (h two) k -> p h two k", two=2)
                i16v = i16f[:].rearrange("p (h two) k -> p h two k", two=2)
                X.op('dve', lambda e, s=s: e.tensor_tensor(out=cand[:], in0=bc(v16v[:, :, 0, :].unsqueeze(3), [128, 8, 16, 16]),
                                                      in1=bc(v16v[:, :, 1, :].unsqueeze(2), [128, 8, 16, 16]), op=ALU.add),
                     reads=['v16'], writes=['cand'])
                for h in range(8):
                    top16(X, cand[:, h, :, :].rearrange("p a b -> p (a b)"), 'cand', s2[:, h * 256:(h + 1) * 256], 's2',
                          ts[:, h, :], tc[:, h, :], 'ts')
                X.op('dve', lambda e, s=s: e.tensor_single_scalar(out=ta[:], in_=tc[:], scalar=4, op=ALU.logical_shift_right),
                     reads=['tsi'], writes=['ta'])
                X.op('dve', lambda e, s=s: e.tensor_single_scalar(out=tb[:], in_=tc[:], scalar=15, op=ALU.bitwise_and),
                     reads=['tsi'], writes=['tb'])
                X.op('dve', lambda e, s=s: e.tensor_copy(out=taf[:], in_=ta[:]), reads=['ta'], writes=['taf'])
                X.op('dve', lambda e, s=s: e.tensor_copy(out=tbf[:], in_=tb[:]), reads=['tb'], writes=['tbf'])
                oh = s_sb[:].rearrange("p (h j a) -> p h j a", h=8, j=16)
                for (sel, half, dst, dtag) in ((taf, 0, i1, 'i1'), (tbf, 1, i2, 'i2')):
                    stag = 'taf' if half == 0 else 'tbf'
                    X.op('dve', lambda e, s=s, sel=sel: e.tensor_tensor(out=oh, in0=bc(sel[:].unsqueeze(3), [128, 8, 16, 16]),
                                                                   in1=bc(iota[:].unsqueeze(1).unsqueeze(1), [128, 8, 16, 16]), op=ALU.is_equal),
                         reads=[stag, 'iota', 's_sb'], writes=['s_sb'])
                    X.op('dve', lambda e, s=s, half=half: e.tensor_tensor(out=oh, in0=oh, in1=bc(i16v[:, :, half, :].unsqueeze(2), [128, 8, 16, 16]),
                                                                     op=ALU.mult), reads=['s_sb', 'i16f'], writes=['s_sb'])
                    X.op('dve', lambda e, s=s, dst=dst: e.reduce_sum(out=dst[:], in_=oh, axis=AX.X), reads=['s_sb'], writes=[dtag])
                X.op('dve', lambda e, s=s: e.scalar_tensor_tensor(out=eidf[:], in0=i1[:].rearrange("p h j -> p (h j)"), scalar=128.0,
                                                             in1=i2[:].rearrange("p h j -> p (h j)"), op0=ALU.mult, op1=ALU.add),
                     reads=['i1', 'i2'], writes=['eidf'])
                X.op('dve', lambda e, s=s: e.tensor_copy(out=eidx[s][:], in_=eidf[:]), reads=['eidf'], writes=[f'eidx{s}'])
                X.op('dve', lambda e, s=s: e.tensor_tensor(out=eg[:], in0=ts[:], in1=bc(ts[:, :, 0:1], [128, 8, 16]), op=ALU.subtract),
                     reads=['ts'], writes=['eg'])
                X.op('act', lambda e, s=s: e.activation(out=eg[:], in_=eg[:], func=AF.Exp), reads=['eg'], writes=['eg'])
                X.op('dve', lambda e, s=s: e.reduce_sum(out=zz[:], in_=eg[:], axis=AX.X), reads=['eg'], writes=['zz'])
                X.op('dve', lambda e, s=s: e.reciprocal(out=zz[:], in_=zz[:]), reads=['zz'], writes=['zz'])
                X.op('dve', lambda e, s=s: e.tensor_tensor(out=gate[s][:], in0=eg[:], in1=bc(zz[:].unsqueeze(2), [128, 8, 16]), op=ALU.mult),
                     reads=['eg', 'zz'], writes=[f'gate{s}'])


            X0 = Deferred(P)
            front(X0, 0)
            X0.flush()
            Bk = None
            for t in range(ntiles):
                s = t % 2
                s3 = t % 3
                Xn = None
                per = 0
                if t + 1 < ntiles:
                    Xn = Deferred(P)
                    front(Xn, t + 1)
                    per = -(-len(Xn.q) // 112)
                perb = -(-len(Bk.q) // 48) if Bk is not None else 0
                for k in range(128):
                    h, j = k // 16, k % 16
                    b, dgi = k % NB, k % NDG
                    P.op('pool', lambda e, s=s, k=k, b=b: e.indirect_dma_start(
                        out=uvb[b][:], out_offset=None, in_=euvb[:, :],
                        in_offset=bass.IndirectOffsetOnAxis(ap=eidx[s][:, k:k + 1], axis=0)),
                        reads=[f'eidx{s}'], writes=[f'uvb{b}'], sem=f'D_uvb{b}', inc=16)
                    P.op('dve', lambda e, s=s, b=b, h=h, j=j: e.scalar_tensor_tensor(
                        out=junk[:], in0=uvb[b][:, 0:D], scalar=1.0, in1=xn2b[s][:], op0=ALU.mult, op1=ALU.mult,
                        accum_out=hcol[:, h, j:j + 1]), reads=[f'uvb{b}', f'xn2b{s}'], writes=['junkd', f'hcol{k}'])
                    P.op('act', lambda e, s=s, h=h, j=j: e.activation(out=actg[:, h, j:j + 1], in_=hcol[:, h, j:j + 1], func=AF.Gelu),
                         reads=[f'hcol{k}'], writes=[f'actg{k}'])
                    P.op('act', lambda e, s=s, h=h, j=j: e.activation(out=actg[:, h, j:j + 1], in_=actg[:, h, j:j + 1], func=AF.Copy,
                                                                      scale=gate[s][:, h, j:j + 1]),
                         reads=[f'actg{k}', f'gate{s}'], writes=[f'actg{k}'])
                    P.op('act', lambda e, s=s, h=h, j=j, dgi=dgi: e.activation(out=dg[dgi][:], in_=ident_f[:], func=AF.Copy,
                                                                               scale=actg[:, h, j:j + 1]),
                         reads=['ident_f', f'actg{k}'], writes=[f'dg{dgi}'])
                    for hf in range(2):
                        P.op('pe', lambda e, s=s, b=b, dgi=dgi, hf=hf, k=k: e.matmul(
                            ps_y[:, hf * 512:(hf + 1) * 512], lhsT=dg[dgi][:], rhs=uvb[b][:, D + hf * 512:D + (hf + 1) * 512],
                            start=(k == 0), stop=(k == 127)),
                            reads=[f'dg{dgi}', f'uvb{b}'], writes=['ps_y'], pe_chain=True)
                    if Bk is not None:
                        Bk.flush(perb)
                    if Xn is not None:
                        Xn.flush(per)
                if Bk is not None:
                    Bk.flush()
                if Xn is not None:
                    Xn.flush()
                P.op('dve', lambda e, s=s: e.tensor_tensor(out=h2[:], in0=ps_y[:], in1=h1t[s][:], op=ALU.add),
                     reads=['ps_y', f'h1t{s}'], writes=['h2'])
                if debug == 'D1':
                    P.dma('sp', 'dbe', dbg_e[:, :], eidx[s][:], reads=[f'eidx{s}'], writes=['dbe'])
                    P.dma('sp', 'dba', dbg_a[:, :], actg[:].rearrange("p h j -> p (h j)"), reads=[f'actg{k}' for k in range(128)], writes=['dba'])
                    P.dma('sp', 'dbh', dbg_h[:, :], hcol[:].rearrange("p h j -> p (h j)"), reads=[f'hcol{k}' for k in range(128)], writes=['dbh'])
                    P.dma('sp', 'dbg', dbg_g[:, :], gate[s][:].rearrange("p h j -> p (h j)"), reads=[f'gate{s}'], writes=['dbg'])
                    P.dma('sp', 'dby', dbg_y[:, :], h2[:], reads=['h2'], writes=['dby'])
                Bn = Deferred(P)
                rstd_d(Bn, 'h2', h2[:], 3, junkb, 'junkb')
                Bn.op('dve', lambda e, s=s, s3=s3: e.tensor_scalar(out=xn3b[:], in0=h2[:], scalar1=sm[5][:, 0:1], scalar2=None, op0=ALU.mult),
                     reads=['h2', 'dsm5'], writes=['xn3b'])
                for c in range(8):
                    Bn.op('pe', lambda e, s=s, s3=s3, c=c: e.transpose(out=ps_tb[:, c, :], in_=xn3b[:, c * 128:(c + 1) * 128], identity=ident_b[:]),
                         reads=['xn3b', 'ident_b'], writes=['psd_tb'], pe_chain=True)
                Bn.op('dve', lambda e, s=s, s3=s3: e.tensor_tensor(out=xn3T[:], in0=ps_tb[:], in1=bc(gple[:].unsqueeze(2), [128, 8, 128]), op=ALU.mult),
                     reads=['psd_tb', 'gple'], writes=['xn3T'])
                for hf in range(2):
                    for c in range(8):
                        Bn.op('pe', lambda e, s=s, s3=s3, c=c, hf=hf: e.matmul(ps_g[:, hf * 512:(hf + 1) * 512], lhsT=xn3T[:, c, :],
                                                                 rhs=Wg[:, c, hf * 512:(hf + 1) * 512], start=(c == 0), stop=(c == 7)),
                             reads=['xn3T', f'Wg{c}'], writes=['ps_g'], pe_chain=True)
                Bn.op('act', lambda e, s=s, s3=s3: e.activation(out=sgm[:], in_=ps_g[:], func=AF.Sigmoid), reads=['ps_g'], writes=['sgm'])
                Bn.op('act', lambda e, s=s, s3=s3: e.activation(out=ptb[:], in_=pt[s3][:], func=AF.Copy), reads=[f'pt{s3}'], writes=['ptb'])
                for c in range(2):
                    Bn.op('pe', lambda e, s=s, s3=s3, c=c: e.transpose(out=ps_tb[:, c, :], in_=ptb[:, c * 128:(c + 1) * 128], identity=ident_b[:]),
                         reads=['ptb', 'ident_b'], writes=['psd_tb'], pe_chain=True)
                Bn.op('act', lambda e, s=s, s3=s3: e.activation(out=pTT[:], in_=ps_tb[:, 0:2, :], func=AF.Copy), reads=['psd_tb'], writes=['pTT'])
                for hf in range(2):
                    for c in range(2):
                        Bn.op('pe', lambda e, s=s, s3=s3, c=c, hf=hf: e.matmul(ps_g[:, hf * 512:(hf + 1) * 512], lhsT=pTT[:, c, :],
                                                                 rhs=Wp[:, c, hf * 512:(hf + 1) * 512], start=(c == 0), stop=(c == 1)),
                             reads=['pTT', f'Wp{c}'], writes=['ps_g'], pe_chain=True)
                Bn.op('dve', lambda e, s=s, s3=s3: e.tensor_tensor(out=sgm[:], in0=ps_g[:], in1=sgm[:], op=ALU.mult),
                     reads=['ps_g', 'sgm'], writes=['sgm'])
                Bn.op('dve', lambda e, s=s, s3=s3: e.tensor_tensor(out=h2[:], in0=h2[:], in1=sgm[:], op=ALU.add), reads=['h2', 'sgm'], writes=['h2'])
                rstd_d(Bn, 'h2', h2[:], 6, junkb, 'junkb')
                Bn.op('dve', lambda e, s=s, s3=s3: e.scalar_tensor_tensor(out=outt[:], in0=h2[:], scalar=sm[8][:, 0:1], in1=fnr[:],
                                                             op0=ALU.mult, op1=ALU.mult), reads=['h2', 'dsm8', 'fnr'], writes=['outt'])
                Bn.dma('sp', 'outd', out_d[t * 128:(t + 1) * 128, :], outt[:], reads=['outt'], writes=['out'])
                Bk = Bn
            if Bk is not None:
                Bk.flush()
            P.emit()
    return nc


def host_inputs(inputs):
    x = np.asarray(inputs["x"], np.float32)
    pos = np.asarray(inputs["positions"], np.int32)
    invf = (np.float32(500000.0) ** (-np.arange(0, 16, 2, dtype=np.float32) / np.float32(16))).astype(np.float32)
    shared = {
        "invf": np.ascontiguousarray(np.broadcast_to(invf[None, :], (128, 8))),
        "ident": np.eye(128, dtype=np.float32),
        "w_in": np.ascontiguousarray(inputs["w_in"][0], np.float32),
        "g_mix": np.ascontiguousarray(np.asarray(inputs["norm_mix"][0], np.float32).reshape(8, 128).T),
    }
    ii = np.arange(128)
    mask2 = np.zeros((128, 2, 128), np.float32)
    mask2[:, 0, :] = (ii[None, :] <= ii[:, None])
    mask2[:, 1, :] = (ii[None, :] >= ii[:, None])
    f32c = lambda a: np.ascontiguousarray(np.asarray(a, np.float32))
    rep = lambda v: f32c(np.broadcast_to(np.asarray(v, np.float32)[None, :], (128, len(v))))
    gyv = np.concatenate([np.asarray(inputs["g_attn_out"][0], np.float32), np.asarray(inputs["g_conv_out"][0], np.float32)])
    shared.update({
        "mask2": mask2,
        "cw": f32c(np.asarray(inputs["conv_w"][0], np.float32).reshape(31, 4, 128).transpose(2, 1, 0)),
        "convb_r": rep(inputs["conv_b"][0]), "lng_r": rep(inputs["conv_ln_g"][0]), "lnb_r": rep(inputs["conv_ln_b"][0]),
        "g_y": f32c(gyv.reshape(8, 128).T),
        "w_out": f32c(inputs["w_out"][0]),
        "w_pq": f32c(inputs["peer_wq"][0]),
        "skT": f32c(np.asarray(inputs["sub_keys"][0], np.float32).transpose(2, 0, 1)),
        "w_g": f32c(inputs["w_ple_gate"][0]),
        "w_p": f32c(inputs["w_ple_proj"][0]),
        "g_ffn_r": rep(inputs["norm_ffn"][0]),
        "g_ple": f32c(np.asarray(inputs["norm_ple"][0], np.float32).reshape(8, 128).T),
        "fn_r": rep(inputs["final_norm"]),
        "iota16": rep(np.arange(16, dtype=np.float32)),
        "euv": np.concatenate([np.asarray(inputs["expert_u"][0], np.float32), np.asarray(inputs["expert_v"][0], np.float32)], axis=1),
    })
    pp = np.asarray(inputs["p"], np.float32)
    maps = []
    for c in range(NCORES):
        b, j = c // 4, c % 4
        xh = np.zeros((NTH * 128, D), np.float32)
        ph = np.zeros((NTH * 128,), np.int32)
        xh[HALO:] = x[b, j * TOK:(j + 1) * TOK]
        ph[HALO:] = pos[b, j * TOK:(j + 1) * TOK]
        if j > 0:
            xh[:HALO] = x[b, j * TOK - HALO:j * TOK]
            ph[:HALO] = pos[b, j * TOK - HALO:j * TOK]
        m = dict(shared)
        m["xh"] = xh
        m["pos_tm"] = np.ascontiguousarray(ph.reshape(NTH, 128).T)
        m["hv"] = np.full((128, 1), 1.0 if j > 0 else 0.0, np.float32)
        m["p_own"] = np.ascontiguousarray(pp[0, b, j * TOK:(j + 1) * TOK])
        maps.append(m)
    return maps


def kernel(**inputs):
    nc = build_program()
    maps = host_inputs(inputs)
    res = run_bass_kernel_spmd(nc, maps, core_ids=list(range(NCORES)))
    out = np.zeros((2, 8192, D), np.float32)
    for c in range(NCORES):
        b, j = c // 4, c % 4
        out[b, j * TOK:(j + 1) * TOK] = res.results[c]["out"]
    return out
```

```python
import contextlib
import numpy as np
import concourse.bass as bass
import concourse.mybir as mybir
from concourse.bass_utils import run_bass_kernel_spmd

F32 = mybir.dt.float32
BF16 = mybir.dt.bfloat16
I32 = mybir.dt.int32
U32 = mybir.dt.uint32
AF = mybir.ActivationFunctionType
ALU = mybir.AluOpType
AX = mybir.AxisListType

NCORES = 8
D = 1024
TOK = 2048
HALO = 2048
NT = TOK // 128
NTH = (TOK + HALO) // 128
EPS = 1e-6
PATTERNS = ((128, 1), (512, 4), (2048, 16))
TWO_PI = 6.283185307179586
C1 = 6.28125
C2 = TWO_PI - C1


class Prog:
    ENGS = ['pe', 'act', 'dve', 'pool', 'sp']

    def __init__(self, nc, tag):
        self.nc = nc
        self.tag = tag
        self.ops = {e: [] for e in self.ENGS}
        self.cnt = {}
        self.lastw = {}
        self.readers = {}
        self.seen = {e: {} for e in self.ENGS}
        self.excl = set()

    def _deps(self, e, reads, writes):
        deps = {}

        def add(d):
            sn, v = d
            if deps.get(sn, 0) < v:
                deps[sn] = v
        for r in reads:
            if r in self.lastw:
                add(self.lastw[r])
        for w in writes:
            if w in self.lastw:
                add(self.lastw[w])
            for rd in self.readers.get(w, ()):
                add(rd)
        out = []
        for sn, v in deps.items():
            if self.seen[e].get(sn, 0) < v:
                self.seen[e][sn] = v
                out.append((sn, v))
        return out

    LIMIT = None
    NOPS = 0
    SEMSTACK = None

    def op(self, e, fn, reads=(), writes=(), sem=None, inc=1, pe_chain=False):
        Prog.NOPS += 1
        if Prog.LIMIT is not None and Prog.NOPS > Prog.LIMIT:
            return None
        semname = sem or ('S_' + e)
        writes = list(writes) + [r for r in reads if r in self.excl and r not in writes]
        waits = self._deps(e, reads, writes)
        if pe_chain:
            waits = [(sn, v) for (sn, v) in waits if sn != 'S_pe']
        self.cnt[semname] = self.cnt.get(semname, 0) + inc
        val = self.cnt[semname]

        def emit(eng, sems, waits=waits, fn=fn, semname=semname, inc=inc):
            for (sn, v) in waits:
                eng.wait_ge(sems[sn], v)
            fn(eng).then_inc(sems[semname], inc)
        self.ops[e].append(emit)
        for w in writes:
            self.lastw[w] = (semname, val)
            self.readers[w] = []
        for r in reads:
            if r not in writes:
                self.readers.setdefault(r, []).append((semname, val))
        return (semname, val)

    def dma(self, q, chan, out, in_, reads=(), writes=(), chain=False, **kw):
        if chain:
            writes = list(writes) + ['chain_' + chan]
        return self.op(q, lambda eng: eng.dma_start(out=out, in_=in_, **kw), reads, writes,
                       sem='D_' + chan, inc=16)

    def emit(self):
        nc = self.nc
        names = sorted(self.cnt.keys())
        with contextlib.ExitStack() as st:
            sems = {n: Prog.SEMSTACK.enter_context(nc.semaphore(self.tag + n)) for n in names}
            block = st.enter_context(nc.Block())
            hooks = {'pe': block.tensor, 'act': block.scalar, 'dve': block.vector,
                     'pool': block.gpsimd, 'sp': block.sync}
            for e in self.ENGS:
                ops = self.ops[e]
                extra = [(n, self.cnt[n]) for n in names]

                def body(eng, ops=ops, extra=extra):
                    for o in ops:
                        o(eng, sems)
                    for (n, v) in extra:
                        eng.wait_ge(sems[n], v)
                hooks[e](body)


class Deferred:
    def __init__(self, P):
        self.P = P
        self.q = []

    def op(self, *a, **k):
        self.q.append(('op', a, k))

    def dma(self, *a, **k):
        self.q.append(('dma', a, k))

    def flush(self, n=None):
        m = len(self.q) if n is None else min(n, len(self.q))
        for kind, a, k in self.q[:m]:
            getattr(self.P, kind)(*a, **k)
        del self.q[:m]


def bc(ap, shape):
    return ap.to_broadcast(list(shape))


def build_program(debug=None):
    nc = bass.Bass("TRN2", target_bir_lowering=False)
    dt_in = lambda name, shape, dt=F32: nc.dram_tensor(name, list(shape), dt, kind="ExternalInput").ap()
    xh = dt_in("xh", [NTH * 128, D])
    pos_tm = dt_in("pos_tm", [128, NTH], I32)
    hv_d = dt_in("hv", [128, 1])
    invf_d = dt_in("invf", [128, 8])
    ident_d = dt_in("ident", [128, 128])
    w_in = dt_in("w_in", [D, 2560])
    g_mix = dt_in("g_mix", [128, 8])
    mask2_d = dt_in("mask2", [128, 2, 128])
    cw_d = dt_in("cw", [128, 4, 31])
    convb_d = dt_in("convb_r", [128, 512])
    lng_d = dt_in("lng_r", [128, 512])
    lnb_d = dt_in("lnb_r", [128, 512])
    g_y = dt_in("g_y", [128, 8])
    w_out = dt_in("w_out", [D, D])
    out_d = nc.dram_tensor("out", [TOK, D], F32, kind="ExternalOutput").ap()
    od = nc.dram_tensor("od", [3, TOK, 520], F32, kind="Internal").ap()
    h1d = nc.dram_tensor("h1d", [TOK, D], F32, kind="Internal").ap()
    if debug == 'B':
        dbg_h1 = nc.dram_tensor("dbg_h1", [TOK, D], F32, kind="ExternalOutput").ap()
    if debug == 'D1':
        dbg_e = nc.dram_tensor("dbg_eidx", [128, 128], I32, kind="ExternalOutput").ap()
        dbg_a = nc.dram_tensor("dbg_actg", [128, 128], F32, kind="ExternalOutput").ap()
        dbg_h = nc.dram_tensor("dbg_hcol", [128, 128], F32, kind="ExternalOutput").ap()
        dbg_g = nc.dram_tensor("dbg_gate", [128, 128], F32, kind="ExternalOutput").ap()
        dbg_y = nc.dram_tensor("dbg_h2", [128, 1024], F32, kind="ExternalOutput").ap()
    p_own = dt_in("p_own", [TOK, 256])
    w_pq = dt_in("w_pq", [D, 2048])
    skT_d = dt_in("skT", [128, 2, 128])
    w_g = dt_in("w_g", [D, D])
    w_p = dt_in("w_p", [256, D])
    g_ffn = dt_in("g_ffn_r", [128, D])
    g_ple = dt_in("g_ple", [128, 8])
    fn_d = dt_in("fn_r", [128, D])
    iota_d = dt_in("iota16", [128, 16])
    euv = dt_in("euv", [16384, 2 * D])
    euvb = nc.dram_tensor("euvb", [16384, 2 * D], BF16, kind="Internal").ap()
    vd = nc.dram_tensor("vd", [NTH * 128, 520], BF16, kind="Internal").ap()
    dbg = {}
    if debug == 'A':
        dbg['qT'] = nc.dram_tensor("dbg_qT", [128, 2, 4, TOK], BF16, kind="ExternalOutput").ap()
        dbg['kT'] = nc.dram_tensor("dbg_kT", [128, 4, NTH * 128], BF16, kind="ExternalOutput").ap()
        dbg['gluT'] = nc.dram_tensor("dbg_gluT", [128, 4, 128 + TOK], BF16, kind="ExternalOutput").ap()
        dbg['vd'] = nc.dram_tensor("dbg_vd", [NTH * 128, 520], BF16, kind="ExternalOutput").ap()

    with contextlib.ExitStack() as outer:
        Prog.SEMSTACK = outer
        sb = lambda name, shape, dt=F32: outer.enter_context(nc.sbuf_tensor(name, list(shape), dt))
        ident_f = sb("ident_f", [128, 128])
        ident_b = sb("ident_b", [128, 128], BF16)
        hv = sb("hv_sb", [128, 1])
        mid = contextlib.ExitStack()
        mid.__enter__()
        sbm = lambda name, shape, dt=F32: mid.enter_context(nc.sbuf_tensor(name, list(shape), dt))
        qT = sbm("qT", [128, 2, 4, TOK], BF16)
        kT = sbm("kT", [128, 4, NTH * 128], BF16)
        gluT = sbm("gluT", [128, 4, 128 + TOK], BF16)

        with contextlib.ExitStack() as pa:
            sa = lambda name, shape, dt=F32: pa.enter_context(nc.sbuf_tensor(name, list(shape), dt))
            P = Prog(nc, "A")

            def ps(name, shape, dt=F32):
                P.excl.add(name)
                return pa.enter_context(nc.psum_tensor(name, list(shape), dt))
            W = sa("W_in_bf", [128, 8, 2560], BF16)
            gmix = sa("gmix", [128, 8])
            pos_i = sa("pos_i", [128, NTH], I32)
            pos_f = sa("pos_f", [128, NTH])
            invf = sa("invf_sb", [128, 8])
            ang = sa("ang", [128, NTH, 8])
            sinT = sa("sinT", [128, NTH, 8])
            cosT = sa("cosT", [128, NTH, 8])
            tmpa = sa("tmpa", [128, NTH, 8])
            tmpn = sa("tmpn", [128, NTH, 8])
            tmpi = sa("tmpi", [128, NTH, 8], I32)
            xt = [sa(f"xt{i}", [128, D]) for i in range(2)]
            junk = sa("junk", [128, D], BF16)
            ss = [sa(f"ss{i}", [128, 1]) for i in range(2)]
            rt = [sa(f"rt{i}", [128, 1]) for i in range(2)]
            rstd = [sa(f"rstd{i}", [128, 1]) for i in range(2)]
            xs = [sa(f"xs{i}", [128, D], BF16) for i in range(2)]
            xnT = [sa(f"xnT{i}", [128, 8, 128], BF16) for i in range(2)]
            zc = sa("zc", [128, 1024])
            zb = sa("zb", [128, 1024], BF16)
            rtmp = [sa(f"rtmp{i}", [128, 16, 8]) for i in range(4)]
            vt = [sa(f"vt{i}", [128, 8, 65], BF16) for i in range(2)]
            sg = sa("sg", [128, 512])
            glu = sa("glu", [128, 512], BF16)
            pst = [ps(f"pst{i}", [128, 8, 128], BF16) for i in range(2)]
            ps_qk = ps("ps_qk", [128, 1024])
            ps_v = ps("ps_v", [128, 512])
            ps_ag = ps("ps_ag", [128, 1024])
            pst2 = ps("pst2", [128, 8, 128], BF16)

            P.op('pool', lambda e: e.memset(qT[:, 0, :, :], 0.0), writes=['qT'])
            P.op('pool', lambda e: e.memset(qT[:, 1, :, :], 0.0), reads=['qT'], writes=['qT'])
            P.dma('sp', 'constA', ident_f[:], ident_d[:, :], writes=['ident_f'], chain=True)
            P.op('dve', lambda e: e.tensor_copy(out=ident_b[:], in_=ident_f[:]), reads=['ident_f'], writes=['ident_b'])
            P.dma('sp', 'constA', hv[:], hv_d[:, :], writes=['hv'], chain=True)
            P.dma('sp', 'constA', gmix[:], g_mix[:, :], writes=['gmix'], chain=True)
            P.dma('sp', 'constA', pos_i[:], pos_tm[:, :], writes=['pos_i'], chain=True)
            P.dma('sp', 'constA', invf[:], invf_d[:, :], writes=['invf'], chain=True)
            for c in range(8):
                for hh in range(2):
                    P.dma('pool', f'Wld{(2 * c + hh) % 4}', W[:, c, hh * 1280:(hh + 1) * 1280],
                          w_in[c * 128:(c + 1) * 128, hh * 1280:(hh + 1) * 1280], writes=[f'W{c}'], chain=True)
            P.op('dve', lambda e: e.tensor_copy(out=pos_f[:], in_=pos_i[:]), reads=['pos_i'], writes=['pos_f'])
            P.op('dve', lambda e: e.tensor_tensor(out=ang[:], in0=bc(pos_f[:].unsqueeze(2), [128, NTH, 8]),
                                                  in1=bc(invf[:].unsqueeze(1), [128, NTH, 8]), op=ALU.mult),
                 reads=['pos_f', 'invf'], writes=['ang'])

            def sin_of(dst, shift, tagn):
                P.op('dve', lambda e: e.tensor_scalar(out=tmpa[:], in0=ang[:], scalar1=float(shift), scalar2=None,
                                                      op0=ALU.add), reads=['ang'], writes=['tmpa'])
                P.op('dve', lambda e: e.tensor_scalar(out=tmpn[:], in0=tmpa[:], scalar1=1.0 / TWO_PI, scalar2=0.5,
                                                      op0=ALU.mult, op1=ALU.add), reads=['tmpa'], writes=['tmpn'])
                P.op('dve', lambda e: e.tensor_copy(out=tmpi[:], in_=tmpn[:]), reads=['tmpn'], writes=['tmpi'])
                P.op('dve', lambda e: e.tensor_copy(out=tmpn[:], in_=tmpi[:]), reads=['tmpi'], writes=['tmpn'])
                P.op('dve', lambda e: e.scalar_tensor_tensor(out=tmpa[:], in0=tmpn[:], scalar=-C1, in1=tmpa[:],
                                                             op0=ALU.mult, op1=ALU.add),
                     reads=['tmpn', 'tmpa'], writes=['tmpa'])
                P.op('dve', lambda e: e.scalar_tensor_tensor(out=tmpa[:], in0=tmpn[:], scalar=-C2, in1=tmpa[:],
                                                             op0=ALU.mult, op1=ALU.add),
                     reads=['tmpn', 'tmpa'], writes=['tmpa'])
                P.op('dve', lambda e: e.tensor_scalar(out=tmpn[:], in0=tmpa[:], scalar1=-np.pi, scalar2=TWO_PI,
                                                      op0=ALU.is_lt, op1=ALU.mult), reads=['tmpa'], writes=['tmpn'])
                P.op('dve', lambda e: e.tensor_tensor(out=tmpa[:], in0=tmpa[:], in1=tmpn[:], op=ALU.add),
                     reads=['tmpa', 'tmpn'], writes=['tmpa'])
                P.op('dve', lambda e: e.tensor_scalar(out=tmpn[:], in0=tmpa[:], scalar1=np.pi, scalar2=-TWO_PI,
                                                      op0=ALU.is_gt, op1=ALU.mult), reads=['tmpa'], writes=['tmpn'])
                P.op('dve', lambda e: e.tensor_tensor(out=tmpa[:], in0=tmpa[:], in1=tmpn[:], op=ALU.add),
                     reads=['tmpa', 'tmpn'], writes=['tmpa'])
                P.op('dve', lambda e: e.tensor_scalar(out=tmpa[:], in0=tmpa[:], scalar1=-3.14159, scalar2=3.14159,
                                                      op0=ALU.max, op1=ALU.min), reads=['tmpa'], writes=['tmpa'])
                P.op('act', lambda e: e.activation(out=dst[:], in_=tmpa[:], func=AF.Sin), reads=['tmpa'], writes=[tagn])
            sin_of(sinT, 0.0, 'sinT')
            sin_of(cosT, np.pi / 2, 'cosT')

            class Router:
                target = None

                def op(self, *a, **k):
                    return self.target.op(*a, **k)

                def dma(self, *a, **k):
                    return self.target.dma(*a, **k)
            R = Router()
            S1 = [Deferred(P) for _ in range(NTH)]
            S2 = [Deferred(P) for _ in range(NTH)]
            def rope(nh, col0, i):
                zv = zc[:, col0:col0 + nh * 64].rearrange("p (h e) -> p h e", e=64)
                bv = zb[:, col0:col0 + nh * 64].rearrange("p (h e) -> p h e", e=64)
                cs = bc(cosT[:, i, :].unsqueeze(1), [128, nh, 8])
                sn = bc(sinT[:, i, :].unsqueeze(1), [128, nh, 8])
                z1, z2 = zv[:, :, 0:8], zv[:, :, 8:16]
                t = [r[:, 0:nh, :] for r in rtmp]
                R.op('pool', lambda e: e.tensor_tensor(out=t[0], in0=z1, in1=cs, op=ALU.mult),
                     reads=['zc', 'cosT'], writes=['rt0'])
                R.op('pool', lambda e: e.tensor_tensor(out=t[1], in0=z2, in1=sn, op=ALU.mult),
                     reads=['zc', 'sinT'], writes=['rt1'])
                R.op('pool', lambda e: e.tensor_tensor(out=t[2], in0=z2, in1=cs, op=ALU.mult),
                     reads=['zc', 'cosT'], writes=['rt2'])
                R.op('pool', lambda e: e.tensor_tensor(out=t[3], in0=z1, in1=sn, op=ALU.mult),
                     reads=['zc', 'sinT'], writes=['rt3'])
                R.op('dve', lambda e: e.tensor_copy(out=zb[:, col0:col0 + nh * 64], in_=zc[:, col0:col0 + nh * 64]),
                     reads=['zc'], writes=['zb'])
                R.op('dve', lambda e: e.tensor_tensor(out=bv[:, :, 0:8], in0=t[0], in1=t[1], op=ALU.subtract),
                     reads=['rt0', 'rt1', 'zb'], writes=['zb'])
                R.op('dve', lambda e: e.tensor_tensor(out=bv[:, :, 8:16], in0=t[2], in1=t[3], op=ALU.add),
                     reads=['rt2', 'rt3', 'zb'], writes=['zb'])

            for i in range(NTH):
                b = i % 2
                R.target = S1[i]
                own = i >= NT
                R.dma('sp', f'xt{b}', xt[b][:], xh[i * 128:(i + 1) * 128, :], writes=[f'xt{b}'])
                R.op('act', lambda e, b=b: e.activation(out=junk[:], in_=xt[b][:], func=AF.Square, accum_out=ss[b][:]),
                     reads=[f'xt{b}'], writes=['junk', f'ss{b}'])
                R.op('act', lambda e, b=b: e.activation(out=rt[b][:], in_=ss[b][:], func=AF.Sqrt, scale=1.0 / D, bias=EPS),
                     reads=[f'ss{b}'], writes=[f'rt{b}'])
                R.op('dve', lambda e, b=b: e.reciprocal(out=rstd[b][:], in_=rt[b][:]), reads=[f'rt{b}'], writes=[f'rstd{b}'])
                R.op('dve', lambda e, b=b: e.tensor_scalar(out=xs[b][:], in0=xt[b][:], scalar1=rstd[b][:, 0:1], scalar2=None,
                                                           op0=ALU.mult), reads=[f'xt{b}', f'rstd{b}'], writes=[f'xs{b}'])
                for c in range(8):
                    R.op('pe', lambda e, b=b, c=c: e.transpose(out=pst[b][:, c, :], in_=xs[b][:, c * 128:(c + 1) * 128],
                                                               identity=ident_b[:]),
                         reads=[f'xs{b}', 'ident_b'], writes=[f'pst{b}'], pe_chain=True)
                R.op('dve', lambda e, b=b: e.tensor_tensor(out=xnT[b][:], in0=pst[b][:],
                                                           in1=bc(gmix[:].unsqueeze(2), [128, 8, 128]), op=ALU.mult),
                     reads=[f'pst{b}', 'gmix'], writes=[f'xnT{b}'])
                R.target = S2[i]

                def slab(psum_ap, col0, tag, b=b):
                    for c in range(8):
                        R.op('pe', lambda e, c=c: e.matmul(psum_ap, lhsT=xnT[b][:, c, :], rhs=W[:, c, col0:col0 + 512],
                                                          start=(c == 0), stop=(c == 7)),
                             reads=[f'xnT{b}', f'W{c}'], writes=[tag], pe_chain=True)
                if own:
                    slab(ps_qk[:, 0:512], 0, 'ps_qk')
                slab(ps_qk[:, 512:1024], 512, 'ps_qk')
                slab(ps_v[:, :], 1024, 'ps_v')
                conv_tile = i >= NT - 1
                if conv_tile:
                    slab(ps_ag[:, 0:512], 1536, 'ps_ag')
                    slab(ps_ag[:, 512:1024], 2048, 'ps_ag')
                lo = 0 if own else 512
                nh = 16 if own else 8
                R.op('act', lambda e, lo=lo: e.copy(out=zc[:, lo:1024], in_=ps_qk[:, lo:1024]), reads=['ps_qk'], writes=['zc'])
                vb = i % 2
                pv3 = ps_v[:, :].rearrange("p (h e) -> p h e", e=64)
                if own:
                    R.op('act', lambda e, vb=vb: e.activation(out=vt[vb][:, :, 0:64], in_=pv3, func=AF.Copy), reads=['ps_v'], writes=[f'vt{vb}'])
                    R.op('pool', lambda e, vb=vb: e.memset(vt[vb][:, :, 64:65], 1.0), reads=[f'vt{vb}'], writes=[f'vt{vb}'])
                else:
                    R.op('act', lambda e, vb=vb: e.activation(out=vt[vb][:, :, 0:64], in_=pv3, func=AF.Copy, scale=hv[:, 0:1]),
                         reads=['ps_v', 'hv'], writes=[f'vt{vb}'])
                    R.op('dve', lambda e, vb=vb: e.tensor_copy(out=vt[vb][:, :, 64:65], in_=bc(hv[:].unsqueeze(1), [128, 8, 1])),
                         reads=[f'vt{vb}', 'hv'], writes=[f'vt{vb}'])
                R.dma('sp', f'vd{vb}', vd[i * 128:(i + 1) * 128, :], vt[vb][:].rearrange("p h e -> p (h e)"),
                      reads=[f'vt{vb}'], writes=['vd'])
                if conv_tile:
                    R.op('act', lambda e: e.activation(out=sg[:], in_=ps_ag[:, 512:1024], func=AF.Sigmoid),
                         reads=['ps_ag'], writes=['sg'])
                    R.op('dve', lambda e: e.tensor_tensor(out=glu[:], in0=ps_ag[:, 0:512], in1=sg[:], op=ALU.mult),
                         reads=['ps_ag', 'sg'], writes=['glu'])
                rope(nh, lo, i)
                c0 = 0 if own else 4
                for c in range(c0, 8):
                    R.op('pe', lambda e, c=c: e.transpose(out=pst2[:, c, :], in_=zb[:, c * 128:(c + 1) * 128], identity=ident_b[:]),
                         reads=['zb', 'ident_b'], writes=['pst2'], pe_chain=True)
                if own:
                    R.op('act', lambda e, i=i: e.activation(out=qT[0:64, 0, :, (i - NT) * 128:(i - NT + 1) * 128], in_=pst2[0:64, 0:4, :], func=AF.Copy),
                         reads=['pst2'], writes=['qT'])
                    R.op('act', lambda e, i=i: e.activation(out=qT[64:128, 1, :, (i - NT) * 128:(i - NT + 1) * 128], in_=pst2[64:128, 0:4, :], func=AF.Copy),
                         reads=['pst2', 'qT'], writes=['qT'])
                R.op('dve', lambda e, i=i: e.tensor_copy(out=kT[:, :, i * 128:(i + 1) * 128], in_=pst2[:, 4:8, :]),
                     reads=['pst2'], writes=['kT'])
                if conv_tile:
                    for c in range(4):
                        R.op('pe', lambda e, c=c: e.transpose(out=pst2[:, c, :], in_=glu[:, c * 128:(c + 1) * 128], identity=ident_b[:]),
                             reads=['glu', 'ident_b'], writes=['pst2'], pe_chain=True)
                    j = i - (NT - 1)
                    R.op('act', lambda e, j=j: e.copy(out=gluT[:, :, j * 128:(j + 1) * 128], in_=pst2[:, 0:4, :]),
                         reads=['pst2'], writes=['gluT'])
            S1[0].flush()
            for i in range(NTH):
                if i + 1 < NTH:
                    S1[i + 1].flush()
                S2[i].flush()
            if debug == 'A':
                P.dma('sp', 'dq', dbg['qT'][:, :, :, :], qT[:], reads=['qT'], writes=['dq'])
                P.dma('sp', 'dk', dbg['kT'][:, :, :], kT[:], reads=['kT'], writes=['dk'])
                P.dma('sp', 'dg', dbg['gluT'][:, :, :], gluT[:], reads=['gluT'], writes=['dg'])
                P.dma('sp', 'dv', dbg['vd'][:, :], vd[:, :], reads=['vd'], writes=['dv'])
            P.emit()
        if debug == 'A':
            mid.__exit__(None, None, None)
            return nc

        with contextlib.ExitStack() as pb:
            sa = lambda name, shape, dt=F32: pb.enter_context(nc.sbuf_tensor(name, list(shape), dt))
            P = Prog(nc, "B")

            def ps(name, shape, dt=F32):
                P.excl.add(name)
                return pb.enter_context(nc.psum_tensor(name, list(shape), dt))
            vp = sa("vp", [128, 32, 520], BF16)
            mask_f = sa("mask_f", [128, 2, 128])
            mask_b = sa("mask_b", [128, 2, 128], BF16)
            e_sb = [sa(f"e_sb{i}", [128, 4, 2, 128], BF16) for i in range(2)]
            pT = [sa(f"pT{i}", [128, 4, 2, 128], BF16) for i in range(2)]
            o_sb = [sa(f"o_sb{i}", [128, 520]) for i in range(2)]
            ps_s = [ps(f"ps_s{i}", [128, 4, 2, 128]) for i in range(2)]
            ps_o = [ps(f"ps_o{i}", [128, 4, 65]) for i in range(2)]
            P.dma('sp', 'mask', mask_f[:], mask2_d[:, :, :], writes=['mask_f'])
            for r in range(16):
                P.dma('pool', 'ecast', euvb[r * 1024:(r + 1) * 1024, :], euv[r * 1024:(r + 1) * 1024, :], writes=['euvb'], chain=True)
            P.op('dve', lambda e: e.tensor_copy(out=mask_b[:], in_=mask_f[:]), reads=['mask_f'], writes=['mask_b'])
            class RouterB:
                target = None

                def op(self, *a, **k):
                    return self.target.op(*a, **k)

                def dma(self, *a, **k):
                    return self.target.dma(*a, **k)
            RB = RouterB()
            stS, stEM, stPV = [], [], []
            blk = 0
            for pi, (wdw, d) in enumerate(PATTERNS):
                NBK = 32 // d
                vsrc = vd.rearrange("(nbk jk d) f -> jk d nbk f", jk=128, d=d)
                vpv = vp[:].rearrange("p (r nbk) f -> p r nbk f", r=d)
                odv = od[pi].rearrange("(nb i d) f -> i d nb f", i=128, d=d)
                vload = Deferred(P)
                RB.target = P if pi == 0 else vload
                if d == 1:
                    for g in range(4):
                        RB.dma('sp', 'vp', vpv[:, 0, g * 8:(g + 1) * 8, :], vsrc[:, 0, g * 8:(g + 1) * 8, :],
                              reads=['vd'], writes=['vp'])
                else:
                    for r in range(d):
                        RB.dma('sp', 'vp', vpv[:, r, :, :], vsrc[:, r, :, :], reads=['vd'], writes=['vp'])
                for r in range(d):
                    for nbq in range(NBK // 2, NBK):
                        ob = blk % 2
                        blk += 1
                        stS.append(Deferred(P))
                        stEM.append(Deferred(P))
                        stPV.append(Deferred(P))
                        if vload is not None and pi > 0:
                            stPV[-1].q.extend(vload.q)
                            vload.q = []
                        vload = None
                        RB.target = stS[-1]
                        qs = (nbq - NBK // 2) * 128 * d + r
                        for half in range(2):
                            for hh in range(4):
                                h = half * 4 + hh
                                c, po = h // 2, 64 * (h % 2)
                                for kb in range(2):
                                    ks = (nbq - 1 + kb) * 128 * d + r
                                    RB.op('pe', lambda e, half=half, hh=hh, kb=kb, c=c, po=po, ks=ks, qs=qs, d=d: e.matmul(
                                        ps_s[half][:, hh, kb, :], lhsT=kT[:, c, ks:ks + 127 * d + 1:d],
                                        rhs=qT[:, po // 64, c, qs:qs + 127 * d + 1:d], start=True, stop=True),
                                        reads=['kT', 'qT'], writes=[f'ps_s{half}'], pe_chain=True)
                        RB.target = stEM[-1]
                        for half in range(2):
                            RB.op('act', lambda e, half=half: e.activation(out=e_sb[half][:], in_=ps_s[half][:], func=AF.Exp, scale=0.125),
                                 reads=[f'ps_s{half}'], writes=[f'e_sb{half}'])
                            RB.op('dve', lambda e, half=half: e.tensor_tensor(out=pT[half][:], in0=e_sb[half][:],
                                                                             in1=bc(mask_b[:].unsqueeze(1), [128, 4, 2, 128]), op=ALU.mult),
                                 reads=[f'e_sb{half}', 'mask_b'], writes=[f'pT{half}'])
                        RB.target = stPV[-1]
                        for half in range(2):
                            for hh in range(4):
                                h = half * 4 + hh
                                for kb in range(2):
                                    RB.op('pe', lambda e, half=half, hh=hh, kb=kb, h=h, r=r, nbq=nbq, vpv=vpv: e.matmul(
                                        ps_o[half][:, hh, :], lhsT=pT[half][:, hh, kb, :],
                                        rhs=vpv[:, r, nbq - 1 + kb, h * 65:(h + 1) * 65], start=(kb == 0), stop=(kb == 1)),
                                        reads=[f'pT{half}', 'vp'], writes=[f'ps_o{half}'], pe_chain=True)
                        for half in range(2):
                            RB.op('act', lambda e, half=half, ob=ob: e.activation(
                                out=o_sb[ob][:, half * 260:(half + 1) * 260], in_=ps_o[half][:].rearrange("p h e -> p (h e)"), func=AF.Copy),
                                reads=[f'ps_o{half}'], writes=[f'o_sb{ob}'])
                        RB.dma('sp', f'od{ob}', odv[:, r, nbq - NBK // 2, :], o_sb[ob][:], reads=[f'o_sb{ob}'], writes=['od'])
            nblk = len(stS)
            stS[0].flush()
            for bi in range(nblk):
                stEM[bi].flush()
                if bi + 1 < nblk:
                    stS[bi + 1].flush()
                stPV[bi].flush()
            P.emit()

        with contextlib.ExitStack() as pb:
            sa = lambda name, shape, dt=F32: pb.enter_context(nc.sbuf_tensor(name, list(shape), dt))
            P = Prog(nc, "C")

            def ps(name, shape, dt=F32):
                P.excl.add(name)
                return pb.enter_context(nc.psum_tensor(name, list(shape), dt))
            Wo = sa("Wo", [128, 8, D], BF16)
            gy = sa("gy", [128, 8])
            cw = sa("cw_sb", [128, 4, 31])
            diagW = sa("diagW", [128, 4, 31, 128], BF16)
            convb = sa("convb", [128, 512])
            lng = sa("lng", [128, 512])
            lnb = sa("lnb", [128, 512])
            o3 = [sa(f"o3_{i}", [128, 3, 520]) for i in range(2)]
            acc = sa("acc", [128, 8, 65])
            rden = sa("rden", [128, 8])
            attn = sa("attn", [128, 8, 64])
            junk2 = sa("junk2", [128, 512], BF16)
            sm = [sa(f"sm{i}", [128, 1]) for i in range(8)]
            yb = sa("yb", [128, D], BF16)
            cs = sa("cs", [128, 512])
            cs2 = sa("cs2", [128, 512])
            bst = sa("bst", [128, 6])
            mv = sa("mv", [128, 2])
            yT = sa("yT", [128, 8, 128], BF16)
            xr = [sa(f"xr{i}", [128, D]) for i in range(2)]
            h1 = [sa(f"h1_{i}", [128, D]) for i in range(2)]
            ps_c = [ps(f"ps_c{i}", [128, 512]) for i in range(2)]
            ps_t = ps("ps_t", [128, 8, 128], BF16)
            ps_h = ps("ps_h", [128, D])
            P.dma('sp', 'constC', gy[:], g_y[:, :], writes=['gy'], chain=True)
            P.dma('sp', 'constC', cw[:], cw_d[:, :, :], writes=['cw'], chain=True)
            P.dma('sp', 'constC', convb[:], convb_d[:, :], writes=['convb'], chain=True)
            P.dma('sp', 'constC', lng[:], lng_d[:, :], writes=['lng'], chain=True)
            P.dma('sp', 'constC', lnb[:], lnb_d[:, :], writes=['lnb'], chain=True)
            for c in range(8):
                P.dma('pool', f'Wold{c % 4}', Wo[:, c, :], w_out[c * 128:(c + 1) * 128, :], writes=[f'Wo{c}'], chain=True)
            for cc in range(4):
                for j in range(31):
                    eng = 'dve' if (j % 2 == 0) else 'pool'
                    P.op(eng, lambda e, cc=cc, j=j: e.tensor_scalar(out=diagW[:, cc, j, :], in0=ident_f[:], scalar1=cw[:, cc, j:j + 1],
                                                                    scalar2=None, op0=ALU.mult),
                         reads=['ident_f', 'cw'], writes=[f'diagW{cc}_{j}'])

            def rstd_of(P, src_tag, src_ap, n, k0):
                P.op('act', lambda e: e.activation(out=junk2[:, 0:n], in_=src_ap, func=AF.Square, accum_out=sm[k0][:]),
                     reads=[src_tag], writes=['junk2', f'sm{k0}'])
                P.op('act', lambda e: e.activation(out=sm[k0 + 1][:], in_=sm[k0][:], func=AF.Sqrt, scale=1.0 / n, bias=EPS),
                     reads=[f'sm{k0}'], writes=[f'sm{k0 + 1}'])
                P.op('dve', lambda e: e.reciprocal(out=sm[k0 + 2][:], in_=sm[k0 + 1][:]), reads=[f'sm{k0 + 1}'], writes=[f'sm{k0 + 2}'])

            def stageC(X, t, which):
                b = t % 2
                if which == 1:
                    X.dma('sp', f'o3_{b}', o3[b][:], od[:, t * 128:(t + 1) * 128, :].rearrange("a p f -> p a f"),
                          reads=['od'], writes=[f'o3_{b}'])
                    X.dma('sp', f'xr{b}', xr[b][:], xh[HALO + t * 128:HALO + (t + 1) * 128, :], writes=[f'xr{b}'])
                    for cc in range(4):
                        for j in range(31):
                            col = 128 + t * 128 - 30 + j
                            X.op('pe', lambda e, cc=cc, j=j, col=col, b=b: e.matmul(ps_c[b][:, cc * 128:(cc + 1) * 128], lhsT=gluT[:, cc, col:col + 128],
                                                                              rhs=diagW[:, cc, j, :], start=(j == 0), stop=(j == 30)),
                                 reads=['gluT', f'diagW{cc}_{j}'], writes=[f'ps_c{b}'], pe_chain=True)

                    return
                accf = acc[:].rearrange("p h e -> p (h e)")
                X.op('dve', lambda e, b=b: e.tensor_tensor(out=accf, in0=o3[b][:, 0, :], in1=o3[b][:, 1, :], op=ALU.add),
                     reads=[f'o3_{b}'], writes=['acc'])
                X.op('dve', lambda e, b=b: e.tensor_tensor(out=accf, in0=accf, in1=o3[b][:, 2, :], op=ALU.add),
                     reads=[f'o3_{b}', 'acc'], writes=['acc'])
                X.op('dve', lambda e: e.reciprocal(out=rden[:], in_=acc[:, :, 64]), reads=['acc'], writes=['rden'])
                X.op('dve', lambda e: e.tensor_tensor(out=attn[:], in0=acc[:, :, 0:64], in1=bc(rden[:].unsqueeze(2), [128, 8, 64]), op=ALU.mult),
                     reads=['acc', 'rden'], writes=['attn'])
                attf = attn[:].rearrange("p h e -> p (h e)")
                rstd_of(X, 'attn', attf, 512, 0)
                X.op('dve', lambda e: e.tensor_scalar(out=yb[:, 0:512], in0=attf, scalar1=sm[2][:, 0:1], scalar2=None, op0=ALU.mult),
                     reads=['attn', 'sm2'], writes=['yb'])
                X.op('dve', lambda e, b=b: e.tensor_tensor(out=cs[:], in0=ps_c[b][:], in1=convb[:], op=ALU.add),
                     reads=[f'ps_c{b}', 'convb'], writes=['cs'])
                X.op('dve', lambda e: e.bn_stats(out=bst[:], in_=cs[:]), reads=['cs'], writes=['bst'])
                X.op('dve', lambda e: e.bn_aggr(out=mv[:], in_=bst[:]), reads=['bst'], writes=['mv'])
                X.op('act', lambda e: e.activation(out=sm[6][:], in_=mv[:, 1:2], func=AF.Sqrt, scale=1.0, bias=EPS),
                     reads=['mv'], writes=['sm6'])
                X.op('dve', lambda e: e.reciprocal(out=sm[7][:], in_=sm[6][:]), reads=['sm6'], writes=['sm7'])
                X.op('dve', lambda e: e.tensor_scalar(out=cs2[:], in0=cs[:], scalar1=mv[:, 0:1], scalar2=sm[7][:, 0:1],
                                                      op0=ALU.subtract, op1=ALU.mult), reads=['cs', 'mv', 'sm7'], writes=['cs2'])
                X.op('pool', lambda e: e.tensor_tensor(out=cs2[:], in0=cs2[:], in1=lng[:], op=ALU.mult), reads=['cs2', 'lng'], writes=['cs2'])
                X.op('pool', lambda e: e.tensor_tensor(out=cs2[:], in0=cs2[:], in1=lnb[:], op=ALU.add), reads=['cs2', 'lnb'], writes=['cs2'])
                X.op('act', lambda e: e.activation(out=cs[:], in_=cs2[:], func=AF.Silu), reads=['cs2'], writes=['cs'])
                rstd_of(X, 'cs', cs[:], 512, 3)
                X.op('dve', lambda e: e.tensor_scalar(out=yb[:, 512:1024], in0=cs[:], scalar1=sm[5][:, 0:1], scalar2=None, op0=ALU.mult),
                     reads=['cs', 'sm5'], writes=['yb'])
                for c in range(8):
                    X.op('pe', lambda e, c=c: e.transpose(out=ps_t[:, c, :], in_=yb[:, c * 128:(c + 1) * 128], identity=ident_b[:]),
                         reads=['yb', 'ident_b'], writes=['ps_t'], pe_chain=True)
                X.op('dve', lambda e: e.tensor_tensor(out=yT[:], in0=ps_t[:], in1=bc(gy[:].unsqueeze(2), [128, 8, 128]), op=ALU.mult),
                     reads=['ps_t', 'gy'], writes=['yT'])
                for hf in range(2):
                    for c in range(8):
                        X.op('pe', lambda e, c=c, hf=hf: e.matmul(ps_h[:, hf * 512:(hf + 1) * 512], lhsT=yT[:, c, :],
                                                                 rhs=Wo[:, c, hf * 512:(hf + 1) * 512], start=(c == 0), stop=(c == 7)),
                             reads=['yT', f'Wo{c}'], writes=['ps_h'], pe_chain=True)
                X.op('dve', lambda e, b=b: e.tensor_tensor(out=h1[b][:], in0=ps_h[:], in1=xr[b][:], op=ALU.add),
                     reads=['ps_h', f'xr{b}'], writes=[f'h1_{b}'])
                X.dma('sp', f'h1d{b}', h1d[t * 128:(t + 1) * 128, :], h1[b][:], reads=[f'h1_{b}'], writes=['h1d'])
            CS1 = [Deferred(P) for _ in range(NT)]
            CS2 = [Deferred(P) for _ in range(NT)]
            for t in range(NT):
                stageC(CS1[t], t, 1)
                stageC(CS2[t], t, 2)
            CS1[0].flush()
            for t in range(NT):
                if t + 1 < NT:
                    CS1[t + 1].flush()
                CS2[t].flush()
            if debug == 'B':
                P.dma('sp', 'dh1', dbg_h1[:, :], h1d[:, :], reads=['h1d'], writes=['dh1'])
            P.emit()
        if debug == 'B':
            mid.__exit__(None, None, None)
            return nc

        mid.__exit__(None, None, None)
        with contextlib.ExitStack() as pd:
            sa = lambda name, shape, dt=F32: pd.enter_context(nc.sbuf_tensor(name, list(shape), dt))
            P = Prog(nc, "D")

            def ps(name, shape, dt=F32):
                P.excl.add(name)
                return pd.enter_context(nc.psum_tensor(name, list(shape), dt))
            NB, NDG = 12, 8
            Wq = sa("Wq", [128, 8, 2048], BF16)
            Wg = sa("Wg", [128, 8, D], BF16)
            Wp = sa("Wp", [128, 2, D], BF16)
            skf = sa("skf", [128, 2, 128])
            skT = sa("skT_sb", [128, 2, 128], BF16)
            gffn = sa("gffn", [128, D])
            fnr = sa("fnr", [128, D])
            gple = sa("gple", [128, 8])
            iota = sa("iota", [128, 16])
            h1t = [sa(f"h1t{i}", [128, D]) for i in range(2)]
            xn2 = sa("xn2", [128, D])
            outt = sa("outt", [128, D])
            junkf = sa("junkf", [128, D], BF16)
            junkb = sa("junkb", [128, D], BF16)
            xn2b = [sa(f"xn2b{i}", [128, D], BF16) for i in range(2)]
            xn2T = sa("xn2T", [128, 8, 128], BF16)
            pqT = sa("pqT", [128, 16, 128], BF16)
            s_sb = sa("s_sb", [128, 2048])
            s2 = sa("s2", [128, 2048])
            cand = sa("cand", [128, 8, 16, 16])
            v16 = sa("v16", [128, 16, 16])
            i16 = sa("i16", [128, 16, 16], U32)
            i16f = sa("i16f", [128, 16, 16])
            ts = sa("ts", [128, 8, 16])
            tc = sa("tc", [128, 8, 16], U32)
            ta = sa("ta", [128, 8, 16], U32)
            tb = sa("tb", [128, 8, 16], U32)
            taf = sa("taf", [128, 8, 16])
            tbf = sa("tbf", [128, 8, 16])
            i1 = sa("i1", [128, 8, 16])
            i2 = sa("i2", [128, 8, 16])
            eidf = sa("eidf", [128, 128])
            eidx = [sa(f"eidx{i}", [128, 128], I32) for i in range(2)]
            eg = sa("eg", [128, 8, 16])
            zz = sa("zz", [128, 8])
            gate = [sa(f"gate{i}", [128, 8, 16]) for i in range(2)]
            hcol = sa("hcol", [128, 8, 16])
            actg = sa("actg", [128, 8, 16])
            sm = [sa(f"dsm{i}", [128, 1]) for i in range(9)]
            junk = sa("junkd", [128, D], BF16)
            uvb = [sa(f"uvb{i}", [128, 2 * D], BF16) for i in range(NB)]
            dg = [sa(f"dg{i}", [128, 128], BF16) for i in range(NDG)]
            h2 = sa("h2", [128, D])
            xn3b = sa("xn3b", [128, D], BF16)
            xn3T = sa("xn3T", [128, 8, 128], BF16)
            sgm = sa("sgm", [128, D])
            pt = [sa(f"pt{i}", [128, 256]) for i in range(3)]
            ptb = sa("ptb", [128, 256], BF16)
            pTT = sa("pTT", [128, 2, 128], BF16)
            ps_t = ps("psd_t", [128, 8, 128], BF16)
            ps_tb = ps("psd_tb", [128, 8, 128], BF16)
            ps_pq = ps("ps_pq", [128, 4, 128])
            ps_sc = ps("ps_sc", [128, 4, 128])
            ps_y = ps("ps_y", [128, D])
            ps_g = ps("ps_g", [128, D])
            P.dma('sp', 'constD', skf[:], skT_d[:, :, :], writes=['skf'], chain=True)
            P.op('dve', lambda e: e.tensor_copy(out=skT[:], in_=skf[:]), reads=['skf'], writes=['skT'])
            P.dma('sp', 'constD', gffn[:], g_ffn[:, :], writes=['gffn'], chain=True)
            P.dma('sp', 'constD', fnr[:], fn_d[:, :], writes=['fnr'], chain=True)
            P.dma('sp', 'constD', gple[:], g_ple[:, :], writes=['gple'], chain=True)
            P.dma('sp', 'constD', iota[:], iota_d[:, :], writes=['iota'], chain=True)
            for c in range(8):
                P.dma('pool', f'WDld{c % 4}', Wq[:, c, :], w_pq[c * 128:(c + 1) * 128, :], writes=[f'Wq{c}'], chain=True)
            for c in range(8):
                P.dma('pool', f'WDld{c % 4}', Wg[:, c, :], w_g[c * 128:(c + 1) * 128, :], writes=[f'Wg{c}'], chain=True)
            for c in range(2):
                P.dma('pool', f'WDld{c % 4}', Wp[:, c, :], w_p[c * 128:(c + 1) * 128, :], writes=[f'Wp{c}'], chain=True)

            def rstd_d(P, src_tag, src_ap, k0, jk, jktag):
                P.op('act', lambda e: e.activation(out=jk[:], in_=src_ap, func=AF.Square, accum_out=sm[k0][:]),
                     reads=[src_tag], writes=[jktag, f'dsm{k0}'])
                P.op('act', lambda e: e.activation(out=sm[k0 + 1][:], in_=sm[k0][:], func=AF.Sqrt, scale=1.0 / D, bias=EPS),
                     reads=[f'dsm{k0}'], writes=[f'dsm{k0 + 1}'])
                P.op('dve', lambda e: e.reciprocal(out=sm[k0 + 2][:], in_=sm[k0 + 1][:]), reads=[f'dsm{k0 + 1}'], writes=[f'dsm{k0 + 2}'])

            def top16(P, src, srctag, scr, scrtag, vout, iout, outtag):
                P.op('dve', lambda e: e.max(out=vout[:, 0:8], in_=src), reads=[srctag], writes=[outtag])
                P.op('dve', lambda e: e.max_index(out=iout[:, 0:8], in_max=vout[:, 0:8], in_values=src),
                     reads=[srctag, outtag], writes=[outtag + 'i'])
                P.op('dve', lambda e: e.match_replace(out=scr, in_to_replace=vout[:, 0:8], in_values=src, imm_value=-1e30),
                     reads=[srctag, outtag], writes=[scrtag])
                P.op('dve', lambda e: e.max(out=vout[:, 8:16], in_=scr), reads=[scrtag, outtag], writes=[outtag])
                P.op('dve', lambda e: e.max_index(out=iout[:, 8:16], in_max=vout[:, 8:16], in_values=scr),
                     reads=[scrtag, outtag, outtag + 'i'], writes=[outtag + 'i'])

            ntiles = {'D1': 1, 'D2': 2, 'D3': 3}.get(debug, NT)
            def front(X, t):
                s = t % 2
                s3 = t % 3
                X.dma('sp', f'h1t{s}', h1t[s][:], h1d[t * 128:(t + 1) * 128, :], reads=['h1d'], writes=[f'h1t{s}'])
                X.dma('sp', f'pt{s3}', pt[s3][:], p_own[t * 128:(t + 1) * 128, :], writes=[f'pt{s3}'])
                rstd_d(X, f'h1t{s}', h1t[s][:], 0, junkf, 'junkf')
                X.op('dve', lambda e, s=s: e.scalar_tensor_tensor(out=xn2[:], in0=h1t[s][:], scalar=sm[2][:, 0:1], in1=gffn[:],
                                                             op0=ALU.mult, op1=ALU.mult), reads=[f'h1t{s}', 'dsm2', 'gffn'], writes=['xn2'])
                X.op('act', lambda e, s=s: e.activation(out=xn2b[s][:], in_=xn2[:], func=AF.Copy), reads=['xn2'], writes=[f'xn2b{s}'])
                for c in range(8):
                    X.op('pe', lambda e, s=s, c=c: e.transpose(out=ps_t[:, c, :], in_=xn2b[s][:, c * 128:(c + 1) * 128], identity=ident_b[:]),
                         reads=[f'xn2b{s}', 'ident_b'], writes=['psd_t'], pe_chain=True)
                X.op('act', lambda e, s=s: e.activation(out=xn2T[:], in_=ps_t[:], func=AF.Copy), reads=['psd_t'], writes=['xn2T'])
                for g in range(4):
                    for q in range(4):
                        hp = g * 4 + q
                        for c in range(8):
                            X.op('pe', lambda e, s=s, c=c, q=q, hp=hp: e.matmul(ps_pq[:, q, :], lhsT=Wq[:, c, hp * 128:(hp + 1) * 128],
                                                                          rhs=xn2T[:, c, :], start=(c == 0), stop=(c == 7)),
                                 reads=['xn2T', f'Wq{c}'], writes=['ps_pq'], pe_chain=True)
                    X.op('act', lambda e, s=s, g=g: e.activation(out=pqT[:, g * 4:(g + 1) * 4, :], in_=ps_pq[:], func=AF.Copy),
                         reads=['ps_pq'], writes=['pqT'])
                    for q in range(4):
                        hp = g * 4 + q
                        X.op('pe', lambda e, s=s, q=q, hp=hp: e.matmul(ps_sc[:, q, :], lhsT=pqT[:, hp, :], rhs=skT[:, hp % 2, :],
                                                                 start=True, stop=True),
                             reads=['pqT', 'skT'], writes=['ps_sc'], pe_chain=True)
                    X.op('act', lambda e, s=s, g=g: e.activation(out=s_sb[:, g * 512:(g + 1) * 512], in_=ps_sc[:].rearrange("p a b -> p (a b)"),
                                                            func=AF.Copy), reads=['ps_sc'], writes=['s_sb'])
                for hp in range(16):
                    top16(X, s_sb[:, hp * 128:(hp + 1) * 128], 's_sb', s2[:, hp * 128:(hp + 1) * 128], 's2',
                          v16[:, hp, :], i16[:, hp, :], 'v16')
                X.op('dve', lambda e, s=s: e.tensor_copy(out=i16f[:], in_=i16[:]), reads=['v16i'], writes=['i16f'])
                v16v = v16[:].rearrange("p (h two) k -> p h two k", two=2)
                i16v = i16f[:].rearrange("p (h two) k -> p h two k", two=2)
                X.op('dve', lambda e, s=s: e.tensor_tensor(out=cand[:], in0=bc(v16v[:, :, 0, :].unsqueeze(3), [128, 8, 16, 16]),
                                                      in1=bc(v16v[:, :, 1, :].unsqueeze(2), [128, 8, 16, 16]), op=ALU.add),
                     reads=['v16'], writes=['cand'])
                for h in range(8):
                    top16(X, cand[:, h, :, :].rearrange("p a b -> p (a b)"), 'cand', s2[:, h * 256:(h + 1) * 256], 's2',
                          ts[:, h, :], tc[:, h, :], 'ts')
                X.op('dve', lambda e, s=s: e.tensor_single_scalar(out=ta[:], in_=tc[:], scalar=4, op=ALU.logical_shift_right),
                     reads=['tsi'], writes=['ta'])
                X.op('dve', lambda e, s=s: e.tensor_single_scalar(out=tb[:], in_=tc[:], scalar=15, op=ALU.bitwise_and),
                     reads=['tsi'], writes=['tb'])
                X.op('dve', lambda e, s=s: e.tensor_copy(out=taf[:], in_=ta[:]), reads=['ta'], writes=['taf'])
                X.op('dve', lambda e, s=s: e.tensor_copy(out=tbf[:], in_=tb[:]), reads=['tb'], writes=['tbf'])
                oh = s_sb[:].rearrange("p (h j a) -> p h j a", h=8, j=16)
                for (sel, half, dst, dtag) in ((taf, 0, i1, 'i1'), (tbf, 1, i2, 'i2')):
                    stag = 'taf' if half == 0 else 'tbf'
                    X.op('dve', lambda e, s=s, sel=sel: e.tensor_tensor(out=oh, in0=bc(sel[:].unsqueeze(3), [128, 8, 16, 16]),
                                                                   in1=bc(iota[:].unsqueeze(1).unsqueeze(1), [128, 8, 16, 16]), op=ALU.is_equal),
                         reads=[stag, 'iota', 's_sb'], writes=['s_sb'])
                    X.op('dve', lambda e, s=s, half=half: e.tensor_tensor(out=oh, in0=oh, in1=bc(i16v[:, :, half, :].unsqueeze(2), [128, 8, 16, 16]),
                                                                     op=ALU.mult), reads=['s_sb', 'i16f'], writes=['s_sb'])
                    X.op('dve', lambda e, s=s, dst=dst: e.reduce_sum(out=dst[:], in_=oh, axis=AX.X), reads=['s_sb'], writes=[dtag])
                X.op('dve', lambda e, s=s: e.scalar_tensor_tensor(out=eidf[:], in0=i1[:].rearrange("p h j -> p (h j)"), scalar=128.0,
                                                             in1=i2[:].rearrange("p h j -> p (h j)"), op0=ALU.mult, op1=ALU.add),
                     reads=['i1', 'i2'], writes=['eidf'])
                X.op('dve', lambda e, s=s: e.tensor_copy(out=eidx[s][:], in_=eidf[:]), reads=['eidf'], writes=[f'eidx{s}'])
                X.op('dve', lambda e, s=s: e.tensor_tensor(out=eg[:], in0=ts[:], in1=bc(ts[:, :, 0:1], [128, 8, 16]), op=ALU.subtract),
                     reads=['ts'], writes=['eg'])
                X.op('act', lambda e, s=s: e.activation(out=eg[:], in_=eg[:], func=AF.Exp), reads=['eg'], writes=['eg'])
                X.op('dve', lambda e, s=s: e.reduce_sum(out=zz[:], in_=eg[:], axis=AX.X), reads=['eg'], writes=['zz'])
                X.op('dve', lambda e, s=s: e.reciprocal(out=zz[:], in_=zz[:]), reads=['zz'], writes=['zz'])
                X.op('dve', lambda e, s=s: e.tensor_tensor(out=gate[s][:], in0=eg[:], in1=bc(zz[:].unsqueeze(2), [128, 8, 16]), op=ALU.mult),
                     reads=['eg', 'zz'], writes=[f'gate{s}'])


            X0 = Deferred(P)
            front(X0, 0)
            X0.flush()
            Bk = None
            for t in range(ntiles):
                s = t % 2
                s3 = t % 3
                Xn = None
                per = 0
                if t + 1 < ntiles:
                    Xn = Deferred(P)
                    front(Xn, t + 1)
                    per = -(-len(Xn.q) // 112)
                perb = -(-len(Bk.q) // 48) if Bk is not None else 0
                for k in range(128):
                    h, j = k // 16, k % 16
                    b, dgi = k % NB, k % NDG
                    P.op('pool', lambda e, s=s, k=k, b=b: e.indirect_dma_start(
                        out=uvb[b][:], out_offset=None, in_=euvb[:, :],
                        in_offset=bass.IndirectOffsetOnAxis(ap=eidx[s][:, k:k + 1], axis=0)),
                        reads=[f'eidx{s}'], writes=[f'uvb{b}'], sem=f'D_uvb{b}', inc=16)
                    P.op('dve', lambda e, s=s, b=b, h=h, j=j: e.scalar_tensor_tensor(
                        out=junk[:], in0=uvb[b][:, 0:D], scalar=1.0, in1=xn2b[s][:], op0=ALU.mult, op1=ALU.mult,
                        accum_out=hcol[:, h, j:j + 1]), reads=[f'uvb{b}', f'xn2b{s}'], writes=['junkd', f'hcol{k}'])
                    P.op('act', lambda e, s=s, h=h, j=j: e.activation(out=actg[:, h, j:j + 1], in_=hcol[:, h, j:j + 1], func=AF.Gelu),
                         reads=[f'hcol{k}'], writes=[f'actg{k}'])
                    P.op('act', lambda e, s=s, h=h, j=j: e.activation(out=actg[:, h, j:j + 1], in_=actg[:, h, j:j + 1], func=AF.Copy,
                                                                      scale=gate[s][:, h, j:j + 1]),
                         reads=[f'actg{k}', f'gate{s}'], writes=[f'actg{k}'])
                    P.op('act', lambda e, s=s, h=h, j=j, dgi=dgi: e.activation(out=dg[dgi][:], in_=ident_f[:], func=AF.Copy,
                                                                               scale=actg[:, h, j:j + 1]),
                         reads=['ident_f', f'actg{k}'], writes=[f'dg{dgi}'])
                    for hf in range(2):
                        P.op('pe', lambda e, s=s, b=b, dgi=dgi, hf=hf, k=k: e.matmul(
                            ps_y[:, hf * 512:(hf + 1) * 512], lhsT=dg[dgi][:], rhs=uvb[b][:, D + hf * 512:D + (hf + 1) * 512],
                            start=(k == 0), stop=(k == 127)),
                            reads=[f'dg{dgi}', f'uvb{b}'], writes=['ps_y'], pe_chain=True)
                    if Bk is not None:
                        Bk.flush(perb)
                    if Xn is not None:
                        Xn.flush(per)
                if Bk is not None:
                    Bk.flush()
                if Xn is not None:
                    Xn.flush()
                P.op('dve', lambda e, s=s: e.tensor_tensor(out=h2[:], in0=ps_y[:], in1=h1t[s][:], op=ALU.add),
                     reads=['ps_y', f'h1t{s}'], writes=['h2'])
                if debug == 'D1':
                    P.dma('sp', 'dbe', dbg_e[:, :], eidx[s][:], reads=[f'eidx{s}'], writes=['dbe'])
                    P.dma('sp', 'dba', dbg_a[:, :], actg[:].rearrange("p h j -> p (h j)"), reads=[f'actg{k}' for k in range(128)], writes=['dba'])
                    P.dma('sp', 'dbh', dbg_h[:, :], hcol[:].rearrange("p h j -> p (h j)"), reads=[f'hcol{k}' for k in range(128)], writes=['dbh'])
                    P.dma('sp', 'dbg', dbg_g[:, :], gate[s][:].rearrange("p h j -> p (h j)"), reads=[f'gate{s}'], writes=['dbg'])
                    P.dma('sp', 'dby', dbg_y[:, :], h2[:], reads=['h2'], writes=['dby'])
                Bn = Deferred(P)
                rstd_d(Bn, 'h2', h2[:], 3, junkb, 'junkb')
                Bn.op('dve', lambda e, s=s, s3=s3: e.tensor_scalar(out=xn3b[:], in0=h2[:], scalar1=sm[5][:, 0:1], scalar2=None, op0=ALU.mult),
                     reads=['h2', 'dsm5'], writes=['xn3b'])
                for c in range(8):
                    Bn.op('pe', lambda e, s=s, s3=s3, c=c: e.transpose(out=ps_tb[:, c, :], in_=xn3b[:, c * 128:(c + 1) * 128], identity=ident_b[:]),
                         reads=['xn3b', 'ident_b'], writes=['psd_tb'], pe_chain=True)
                Bn.op('dve', lambda e, s=s, s3=s3: e.tensor_tensor(out=xn3T[:], in0=ps_tb[:], in1=bc(gple[:].unsqueeze(2), [128, 8, 128]), op=ALU.mult),
                     reads=['psd_tb', 'gple'], writes=['xn3T'])
                for hf in range(2):
                    for c in range(8):
                        Bn.op('pe', lambda e, s=s, s3=s3, c=c, hf=hf: e.matmul(ps_g[:, hf * 512:(hf + 1) * 512], lhsT=xn3T[:, c, :],
                                                                 rhs=Wg[:, c, hf * 512:(hf + 1) * 512], start=(c == 0), stop=(c == 7)),
                             reads=['xn3T', f'Wg{c}'], writes=['ps_g'], pe_chain=True)
                Bn.op('act', lambda e, s=s, s3=s3: e.activation(out=sgm[:], in_=ps_g[:], func=AF.Sigmoid), reads=['ps_g'], writes=['sgm'])
                Bn.op('act', lambda e, s=s, s3=s3: e.activation(out=ptb[:], in_=pt[s3][:], func=AF.Copy), reads=[f'pt{s3}'], writes=['ptb'])
                for c in range(2):
                    Bn.op('pe', lambda e, s=s, s3=s3, c=c: e.transpose(out=ps_tb[:, c, :], in_=ptb[:, c * 128:(c + 1) * 128], identity=ident_b[:]),
                         reads=['ptb', 'ident_b'], writes=['psd_tb'], pe_chain=True)
                Bn.op('act', lambda e, s=s, s3=s3: e.activation(out=pTT[:], in_=ps_tb[:, 0:2, :], func=AF.Copy), reads=['psd_tb'], writes=['pTT'])
                for hf in range(2):
                    for c in range(2):
                        Bn.op('pe', lambda e, s=s, s3=s3, c=c, hf=hf: e.matmul(ps_g[:, hf * 512:(hf + 1) * 512], lhsT=pTT[:, c, :],
                                                                 rhs=Wp[:, c, hf * 512:(hf + 1) * 512], start=(c == 0), stop=(c == 1)),
                             reads=['pTT', f'Wp{c}'], writes=['ps_g'], pe_chain=True)
                Bn.op('dve', lambda e, s=s, s3=s3: e.tensor_tensor(out=sgm[:], in0=ps_g[:], in1=sgm[:], op=ALU.mult),
                     reads=['ps_g', 'sgm'], writes=['sgm'])
                Bn.op('dve', lambda e, s=s, s3=s3: e.tensor_tensor(out=h2[:], in0=h2[:], in1=sgm[:], op=ALU.add), reads=['h2', 'sgm'], writes=['h2'])
                rstd_d(Bn, 'h2', h2[:], 6, junkb, 'junkb')
                Bn.op('dve', lambda e, s=s, s3=s3: e.scalar_tensor_tensor(out=outt[:], in0=h2[:], scalar=sm[8][:, 0:1], in1=fnr[:],
                                                             op0=ALU.mult, op1=ALU.mult), reads=['h2', 'dsm8', 'fnr'], writes=['outt'])
                Bn.dma('sp', 'outd', out_d[t * 128:(t + 1) * 128, :], outt[:], reads=['outt'], writes=['out'])
                Bk = Bn
            if Bk is not None:
                Bk.flush()
            P.emit()
    return nc


def host_inputs(inputs):
    x = np.asarray(inputs["x"], np.float32)
    pos = np.asarray(inputs["positions"], np.int32)
    invf = (np.float32(500000.0) ** (-np.arange(0, 16, 2, dtype=np.float32) / np.float32(16))).astype(np.float32)
    shared = {
        "invf": np.ascontiguousarray(np.broadcast_to(invf[None, :], (128, 8))),
        "ident": np.eye(128, dtype=np.float32),
        "w_in": np.ascontiguousarray(inputs["w_in"][0], np.float32),
        "g_mix": np.ascontiguousarray(np.asarray(inputs["norm_mix"][0], np.float32).reshape(8, 128).T),
    }
    ii = np.arange(128)
    mask2 = np.zeros((128, 2, 128), np.float32)
    mask2[:, 0, :] = (ii[None, :] <= ii[:, None])
    mask2[:, 1, :] = (ii[None, :] >= ii[:, None])
    f32c = lambda a: np.ascontiguousarray(np.asarray(a, np.float32))
    rep = lambda v: f32c(np.broadcast_to(np.asarray(v, np.float32)[None, :], (128, len(v))))
    gyv = np.concatenate([np.asarray(inputs["g_attn_out"][0], np.float32), np.asarray(inputs["g_conv_out"][0], np.float32)])
    shared.update({
        "mask2": mask2,
        "cw": f32c(np.asarray(inputs["conv_w"][0], np.float32).reshape(31, 4, 128).transpose(2, 1, 0)),
        "convb_r": rep(inputs["conv_b"][0]), "lng_r": rep(inputs["conv_ln_g"][0]), "lnb_r": rep(inputs["conv_ln_b"][0]),
        "g_y": f32c(gyv.reshape(8, 128).T),
        "w_out": f32c(inputs["w_out"][0]),
        "w_pq": f32c(inputs["peer_wq"][0]),
        "skT": f32c(np.asarray(inputs["sub_keys"][0], np.float32).transpose(2, 0, 1)),
        "w_g": f32c(inputs["w_ple_gate"][0]),
        "w_p": f32c(inputs["w_ple_proj"][0]),
        "g_ffn_r": rep(inputs["norm_ffn"][0]),
        "g_ple": f32c(np.asarray(inputs["norm_ple"][0], np.float32).reshape(8, 128).T),
        "fn_r": rep(inputs["final_norm"]),
        "iota16": rep(np.arange(16, dtype=np.float32)),
        "euv": np.concatenate([np.asarray(inputs["expert_u"][0], np.float32), np.asarray(inputs["expert_v"][0], np.float32)], axis=1),
    })
    pp = np.asarray(inputs["p"], np.float32)
    maps = []
    for c in range(NCORES):
        b, j = c // 4, c % 4
        xh = np.zeros((NTH * 128, D), np.float32)
        ph = np.zeros((NTH * 128,), np.int32)
        xh[HALO:] = x[b, j * TOK:(j + 1) * TOK]
        ph[HALO:] = pos[b, j * TOK:(j + 1) * TOK]
        if j > 0:
            xh[:HALO] = x[b, j * TOK - HALO:j * TOK]
            ph[:HALO] = pos[b, j * TOK - HALO:j * TOK]
        m = dict(shared)
        m["xh"] = xh
        m["pos_tm"] = np.ascontiguousarray(ph.reshape(NTH, 128).T)
        m["hv"] = np.full((128, 1), 1.0 if j > 0 else 0.0, np.float32)
        m["p_own"] = np.ascontiguousarray(pp[0, b, j * TOK:(j + 1) * TOK])
        maps.append(m)
    return maps


def kernel(**inputs):
    nc = build_program()
    maps = host_inputs(inputs)
    res = run_bass_kernel_spmd(nc, maps, core_ids=list(range(NCORES)))
    out = np.zeros((2, 8192, D), np.float32)
    for c in range(NCORES):
        b, j = c // 4, c % 4
        out[b, j * TOK:(j + 1) * TOK] = res.results[c]["out"]
    return out
```
